# Optimizing a Trainium2 kernel written in Bass

```python
import jax, jax.numpy as jnp
from jax import lax
import numpy as np

D_MODEL = 2048
BATCH = 4
SEQ = 2048
DEPTH = 1
DEC_BATCH = 128
DEC_SEQ = 4
PAST_LEN = 16384
PAGE_SIZE = 128

D_MIX = D_MODEL
CONV_CH = D_MIX // 2
HGRN_WIDTH = D_MIX - CONV_CH
CONV_WIDTH = 31
HGRN_HEAD_K = 128
HGRN_HEAD_V = 128
HGRN_HEADS = HGRN_WIDTH // HGRN_HEAD_K
HGRN_CHUNK = 64
N_GROUPS = 4
EXPERTS_PER_GROUP = 8
N_EXPERTS = N_GROUPS * EXPERTS_PER_GROUP
TOP_K_IN_GROUP = 2
D_EXPERT = D_MODEL // 4
EPS = 1e-6
IN_COLS = 2 * CONV_CH + 2 * HGRN_HEADS * HGRN_HEAD_K + 2 * HGRN_HEADS * HGRN_HEAD_V

kernel_name = "hybrid_conformerconv_hgrn2_hmoe_step"


def _rmsnorm(x, g):
    xf = x.astype(jnp.float32)
    y = xf * lax.rsqrt(jnp.mean(xf * xf, axis=-1, keepdims=True) + EPS)
    return (y * g.astype(jnp.float32)).astype(x.dtype)


def _layernorm(x, g, b):
    xf = x.astype(jnp.float32)
    mu = jnp.mean(xf, axis=-1, keepdims=True)
    var = jnp.mean(jnp.square(xf - mu), axis=-1, keepdims=True)
    y = (xf - mu) * lax.rsqrt(var + EPS)
    return (y * g.astype(jnp.float32) + b.astype(jnp.float32)).astype(x.dtype)


def _hgrn2_recurrence(q, k, v, logf, s0):
    B, H, T, DK = q.shape
    DV = v.shape[-1]
    C = HGRN_CHUNK if T % HGRN_CHUNK == 0 else T
    n = T // C

    def to_chunks(t):
        return jnp.moveaxis(t.reshape(B, H, n, C, t.shape[-1]), 2, 0)

    causal = jnp.tril(jnp.ones((C, C), dtype=bool))[:, :, None]

    def step(S, inp):
        qc, kc, vc, lfc = inp
        bc = jnp.cumsum(lfc, axis=-2)
        rel = bc[..., :, None, :] - bc[..., None, :, :]
        decay = jnp.where(causal, jnp.exp(jnp.where(causal, rel, 0.0)), 0.0)
        att = jnp.einsum('bhtd,bhsd,bhtsd->bhts', qc, kc, decay)
        o = jnp.einsum('bhts,bhsv->bhtv', att, vc) + jnp.einsum('bhtd,bhdv->bhtv', qc * jnp.exp(bc), S)
        blast = bc[..., -1:, :]
        S_new = jnp.exp(blast[..., 0, :])[..., None] * S + jnp.einsum('bhsd,bhsv->bhdv', kc * jnp.exp(blast - bc), vc)
        return S_new, o

    S_T, o = lax.scan(step, s0, (to_chunks(q), to_chunks(k), to_chunks(v), to_chunks(logf)))
    o = jnp.moveaxis(o, 0, 2).reshape(B, H, T, DV)
    return o, S_T


def _mixer(h, conv_buf, s0, w_in, w_dw, b_dw, ln_g, ln_b, lb, g_norm, w_out):
    B, T, _ = h.shape
    z = jnp.einsum('btd,dc->btc', h, w_in)
    c1 = CONV_CH
    c2 = 2 * CONV_CH
    c3 = c2 + HGRN_HEADS * HGRN_HEAD_K
    c4 = c3 + HGRN_HEADS * HGRN_HEAD_K
    c5 = c4 + HGRN_HEADS * HGRN_HEAD_V
    a, ga, q, f, i, g = jnp.split(z, [c1, c2, c3, c4, c5], axis=-1)

    u = a * jax.nn.sigmoid(ga)
    ext = jnp.concatenate([conv_buf.astype(u.dtype), u], axis=1)
    c = lax.conv_general_dilated(ext, w_dw[:, None, :].astype(ext.dtype), (1,), 'VALID',
                                 dimension_numbers=('NWC', 'WIO', 'NWC'),
                                 feature_group_count=CONV_CH) + b_dw.astype(ext.dtype)
    new_buf = ext[:, -(CONV_WIDTH - 1):]
    c = jax.nn.silu(_layernorm(c, ln_g, ln_b))

    def heads(t, d):
        return t.reshape(B, T, HGRN_HEADS, d).transpose(0, 2, 1, 3)
    fg = lb + (1.0 - lb) * jax.nn.sigmoid(f.astype(jnp.float32))
    qh = heads(jax.nn.silu(q.astype(jnp.float32)), HGRN_HEAD_K)
    kh = heads(1.0 - fg, HGRN_HEAD_K)
    lfh = heads(jnp.log(fg), HGRN_HEAD_K)
    vh = heads(i.astype(jnp.float32), HGRN_HEAD_V)
    o, s_new = _hgrn2_recurrence(qh, kh, vh, lfh, s0.astype(jnp.float32))
    o = _rmsnorm(o, g_norm)
    o = o.transpose(0, 2, 1, 3).reshape(B, T, HGRN_HEADS * HGRN_HEAD_V).astype(h.dtype) * jax.nn.silu(g)

    y = jnp.einsum('btc,cd->btd', jnp.concatenate([c, o], axis=-1), w_out)
    return y, new_buf, s_new


def _hier_moe(h, w_rg, b_rg, w_re, b_re, w_gate, w_up, w_down):
    B, T, D = h.shape
    x = h.reshape(B * T, D)
    gprob = jax.nn.softmax((x @ w_rg + b_rg).astype(jnp.float32), axis=-1)
    p_top, g_idx = lax.top_k(gprob, 1)
    elog = (x @ w_re + b_re).astype(jnp.float32).reshape(-1, N_GROUPS, EXPERTS_PER_GROUP)
    elog_sel = jnp.take_along_axis(elog, g_idx[:, :, None], axis=1)[:, 0]
    e_val, e_idx = lax.top_k(elog_sel, TOP_K_IN_GROUP)
    e_w = jax.nn.softmax(e_val, axis=-1) * p_top
    ids = g_idx * EXPERTS_PER_GROUP + e_idx
    combine = jnp.sum(jax.nn.one_hot(ids, N_EXPERTS, dtype=jnp.float32) * e_w[..., None], axis=1)
    hid = jax.nn.silu(jnp.einsum('nd,edf->nef', x, w_gate)) * jnp.einsum('nd,edf->nef', x, w_up)
    hid = hid * combine[..., None].astype(hid.dtype)
    y = jnp.einsum('nef,efd->nd', hid, w_down)
    return y.reshape(B, T, D)


def setup_inputs(seed: int = 0) -> dict:
    key = jax.random.key(seed)
    ks = jax.random.split(key, 24)

    def nrm(k, shape, scale):
        return jax.random.normal(k, shape, jnp.float32) * scale

    return {
        'x_prompt': nrm(ks[0], (BATCH, SEQ, D_MODEL), 1.0),
        'x_sample': nrm(ks[1], (DEC_BATCH, DEC_SEQ, D_MODEL), 1.0),
        'state_conv': nrm(ks[2], (DEPTH, DEC_BATCH, CONV_WIDTH - 1, CONV_CH), 0.5),
        'state_hgrn': nrm(ks[3], (DEPTH, DEC_BATCH, HGRN_HEADS, HGRN_HEAD_K, HGRN_HEAD_V), 0.1),
        'norm_mix': 1.0 + nrm(ks[4], (DEPTH, D_MODEL), 0.02),
        'w_in': nrm(ks[5], (DEPTH, D_MODEL, IN_COLS), D_MODEL ** -0.5),
        'w_dw': nrm(ks[6], (DEPTH, CONV_WIDTH, CONV_CH), CONV_WIDTH ** -0.5),
        'b_dw': nrm(ks[7], (DEPTH, CONV_CH), 0.02),
        'ln_conv_g': 1.0 + nrm(ks[8], (DEPTH, CONV_CH), 0.02),
        'ln_conv_b': nrm(ks[9], (DEPTH, CONV_CH), 0.02),
        'lb_logits': nrm(ks[10], (DEPTH + 1, HGRN_HEADS * HGRN_HEAD_K), 0.5),
        'hgrn_norm_g': 1.0 + nrm(ks[11], (DEPTH, HGRN_HEAD_V), 0.02),
        'w_out': nrm(ks[12], (DEPTH, D_MIX, D_MODEL), D_MIX ** -0.5),
        'norm_ffn': 1.0 + nrm(ks[13], (DEPTH, D_MODEL), 0.02),
        'w_router_group': nrm(ks[14], (DEPTH, D_MODEL, N_GROUPS), D_MODEL ** -0.5),
        'b_router_group': nrm(ks[15], (DEPTH, N_GROUPS), 0.01),
        'w_router_expert': nrm(ks[16], (DEPTH, D_MODEL, N_EXPERTS), D_MODEL ** -0.5),
        'b_router_expert': nrm(ks[17], (DEPTH, N_EXPERTS), 0.01),
        'w_exp_gate': nrm(ks[18], (DEPTH, N_EXPERTS, D_MODEL, D_EXPERT), D_MODEL ** -0.5),
        'w_exp_up': nrm(ks[19], (DEPTH, N_EXPERTS, D_MODEL, D_EXPERT), D_MODEL ** -0.5),
        'w_exp_down': nrm(ks[20], (DEPTH, N_EXPERTS, D_EXPERT, D_MODEL), D_EXPERT ** -0.5),
        'norm_final': 1.0 + nrm(ks[21], (D_MODEL,), 0.02),
    }


def reference(x_prompt, x_sample, state_conv, state_hgrn, norm_mix, w_in, w_dw, b_dw, ln_conv_g, ln_conv_b,
              lb_logits, hgrn_norm_g, w_out, norm_ffn, w_router_group, b_router_group, w_router_expert,
              b_router_expert, w_exp_gate, w_exp_up, w_exp_down, norm_final):
    lb_all = jnp.cumsum(jax.nn.softmax(lb_logits.astype(jnp.float32), axis=0), axis=0)
    xp, xs = x_prompt, x_sample
    conv_p, hgrn_p, conv_s, hgrn_s = [], [], [], []
    for l in range(DEPTH):
        mix_w = (w_in[l], w_dw[l], b_dw[l], ln_conv_g[l], ln_conv_b[l], lb_all[l], hgrn_norm_g[l], w_out[l])
        moe_w = (w_router_group[l], b_router_group[l], w_router_expert[l], b_router_expert[l],
                 w_exp_gate[l], w_exp_up[l], w_exp_down[l])
        buf0 = jnp.zeros((xp.shape[0], CONV_WIDTH - 1, CONV_CH), xp.dtype)
        s00 = jnp.zeros((xp.shape[0], HGRN_HEADS, HGRN_HEAD_K, HGRN_HEAD_V), jnp.float32)
        yp, bp, sp = _mixer(_rmsnorm(xp, norm_mix[l]), buf0, s00, *mix_w)
        xp = xp + yp
        xp = xp + _hier_moe(_rmsnorm(xp, norm_ffn[l]), *moe_w)
        ys, bs, ss = _mixer(_rmsnorm(xs, norm_mix[l]), state_conv[l], state_hgrn[l], *mix_w)
        xs = xs + ys
        xs = xs + _hier_moe(_rmsnorm(xs, norm_ffn[l]), *moe_w)
        conv_p.append(bp)
        hgrn_p.append(sp)
        conv_s.append(bs)
        hgrn_s.append(ss)
    y_prompt = _rmsnorm(xp, norm_final)
    y_sample = _rmsnorm(xs, norm_final)
    new_conv_prompt = jnp.stack(conv_p, axis=0)
    new_hgrn_prompt = jnp.stack(hgrn_p, axis=0)
    new_conv_sample = jnp.stack(conv_s, axis=0)
    new_hgrn_sample = jnp.stack(hgrn_s, axis=0)
    return (y_prompt, y_sample, new_conv_prompt, new_hgrn_prompt, new_conv_sample, new_hgrn_sample)
```

```python
import numpy as np
import ml_dtypes
from contextlib import ExitStack
import concourse.bass as bass
import concourse.mybir as mybir
from concourse.bass_utils import run_bass_kernel_spmd

F32 = mybir.dt.float32
BF16 = mybir.dt.bfloat16
AF = mybir.ActivationFunctionType
ALU = mybir.AluOpType
AX = mybir.AxisListType

NCORES = 8
D = 2048
NK = 16
TCUR = 1024
TS = 64
THALO = 32
TV = TCUR + TS
TM = TV + THALO
TPREV = 1024
CONVW = 31
NE = 32
DE = 512
EPS = 1e-6
CAP = 128
NPRE = 16
BIGSLOT = 1.0e6
FORCE_FALLBACK = False
I32 = mybir.dt.int32
NPV = 16 + 16 + 8 + 8 + 8 + 8 + 8 + 1 + 8 * CONVW
PV_GMIX, PV_GFFN, PV_BDW, PV_LNG, PV_LNB, PV_L0, PV_L1, PV_GN, PV_WDW = 0, 16, 32, 40, 48, 56, 64, 72, 73


class Tk:
    __slots__ = ("sem", "name", "val", "eng")

    def __init__(self, sem, name, val, eng):
        self.sem, self.name, self.val, self.eng = sem, name, val, eng


class T:
    __slots__ = ("w", "r", "name")

    REG = []

    def __init__(self, name=""):
        self.w = None
        self.r = []
        self.name = name
        T.REG.append(self)


class _Dummy:
    def then_inc(self, *a, **kw):
        return self


class _Rec:
    def __init__(self):
        self.calls = []

    def __getattr__(self, name):
        def f(*a, **kw):
            self.calls.append((name, a, kw))
            return _Dummy()
        return f


class K:
    NDS = 12
    defer = None

    def __init__(self, nc, es):
        self.nc = nc
        self.eng = {"pe": nc.tensor, "act": nc.scalar, "dve": nc.vector, "pool": nc.gpsimd, "sp": nc.sync}
        self.sem = {}
        self.cnt = {}
        self.seen = {e: {} for e in self.eng}
        for e in ("pe", "act", "dve", "pool"):
            self.sem[e] = es.enter_context(nc.semaphore("s_" + e))
            self.cnt[e] = 0
        self.dsem = {}
        self.dcnt = {}
        self.dnext = {}
        for q in ("sp", "pool", "act", "poolbg", "spbg"):
            n = self.NDS if q not in ("act",) else 4
            self.dsem[q] = [es.enter_context(nc.semaphore("d_%s%d" % (q, i))) for i in range(n)]
            self.dcnt[q] = [0] * n
            self.dnext[q] = 0

    def _wait(self, e, tks):
        seen = self.seen[e]
        for tk in tks:
            if tk is None:
                continue
            if e == "pe" and tk.eng == "pe":
                continue
            if seen.get(tk.name, 0) >= tk.val:
                continue
            if self.defer is not None:
                self.defer[e].append(("w", tk.sem, tk.val))
            else:
                self.eng[e].wait_ge(tk.sem, tk.val)
            seen[tk.name] = tk.val

    def _deps(self, reads, writes):
        deps = []
        for t in reads:
            deps.append(t.w)
        for t in writes:
            deps.append(t.w)
            deps.extend(t.r)
        return deps

    def snapshot(self):
        return ({e: c for e, c in self.cnt.items()}, {q: list(v) for q, v in self.dcnt.items()}, dict(self.dnext),
                {e: dict(v) for e, v in self.seen.items()}, [(t, t.w, list(t.r)) for t in T.REG])

    def restore(self, snap):
        cnt, dcnt, dnext, seen, tiles = snap
        self.cnt = dict(cnt)
        self.dcnt = {q: list(v) for q, v in dcnt.items()}
        self.dnext = dict(dnext)
        self.seen = {e: dict(v) for e, v in seen.items()}
        for (t, w, r) in tiles:
            t.w = w
            t.r = list(r)

    def start_defer(self):
        self.defer = {e: [] for e in self.eng}

    def end_defer(self):
        d = self.defer
        self.defer = None
        return d

    def replay(self, e, items):
        eng = self.eng[e]
        if not items:
            eng.nop()
        for it in items:
            if it[0] == "w":
                eng.wait_ge(it[1], it[2])
            else:
                inst = None
                for (name, a, kw) in it[1]:
                    inst = getattr(eng, name)(*a, **kw)
                inst.then_inc(it[2], it[3])

    def soft_barrier(self):
        for e in self.eng:
            tks = [Tk(self.sem[x], x, self.cnt[x], x) for x in self.sem if self.cnt[x] > 0 and x != e]
            self._wait(e, tks)
            self.drain_dmas(e)

    def op(self, e, fn, reads=(), writes=()):
        self._wait(e, [self.gconst.w] if getattr(self, "gconst", None) is not None else [])
        self._wait(e, self._deps(reads, writes))
        if self.defer is not None:
            rec = _Rec()
            fn(rec)
            self.defer[e].append(("i", rec.calls, self.sem[e], 1))
        else:
            inst = fn(self.eng[e])
            inst.then_inc(self.sem[e], 1)
        self.cnt[e] += 1
        tk = Tk(self.sem[e], e, self.cnt[e], e)
        for t in reads:
            t.r.append(tk)
        for t in writes:
            t.w = tk
            t.r = []
        return tk

    def dma(self, q, out, in_, reads=(), writes=(), fn=None, bg=False, **kw):
        sq = q + "bg" if bg else q
        self._wait(q, [self.gconst.w] if getattr(self, "gconst", None) is not None else [])
        self._wait(q, self._deps(reads, writes))
        i = self.dnext[sq]
        self.dnext[sq] = (i + 1) % len(self.dsem[sq])
        sem = self.dsem[sq][i]
        name = "d_%s%d" % (sq, i)
        if self.dcnt[sq][i] > 0:
            self._wait(q, [Tk(sem, name, self.dcnt[sq][i], "dma")])
        if self.defer is not None:
            rec = _Rec()
            if fn is not None:
                fn(rec)
            else:
                rec.dma_start(out=out, in_=in_, **kw)
            self.defer[q].append(("i", rec.calls, sem, 16))
        elif fn is not None:
            fn(self.eng[q]).then_inc(sem, 16)
        else:
            self.eng[q].dma_start(out=out, in_=in_, **kw).then_inc(sem, 16)
        self.dcnt[sq][i] += 16
        tk = Tk(sem, name, self.dcnt[sq][i], "dma")
        for t in reads:
            t.r.append(tk)
        for t in writes:
            t.w = tk
            t.r = []
        return tk

    def drain_dmas(self, e="sp", bg=False):
        for q in self.dsem:
            if q.endswith("bg") and not bg:
                continue
            for i, sem in enumerate(self.dsem[q]):
                if self.dcnt[q][i] > 0:
                    self._wait(e, [Tk(sem, "d_%s%d" % (q, i), self.dcnt[q][i], "dma")])

    def phase_end(self):
        self.drain_dmas("sp")
        self.nc.all_engine_barrier()


def build_program(stage=9):
    nc = bass.Bass("TRN2", target_bir_lowering=False)

    def din(name, shape):
        return nc.dram_tensor(name, list(shape), F32, kind="ExternalInput").ap()

    def dout(name, shape):
        return nc.dram_tensor(name, list(shape), F32, kind="ExternalOutput").ap()

    x_main = din("x_main", [TM, D])
    x_prev = din("x_prev", [TPREV, D])
    sconv = din("sconv", [16, 30, 1024])
    shgrn = din("shgrn", [16, 8, 128, 128])
    pvec = din("pvec", [128, NPV])
    nfin = din("nfin", [128, D])
    gffn_rep = din("gffn_rep", [128, D])
    zeros_d = nc.dram_tensor("zeros_d", [128, D], BF16, kind="ExternalInput").ap()
    w_in = din("w_in", [D, 6144])
    w_out = din("w_out", [D, D])
    w_r = din("w_r", [D, 36])
    b_r = din("b_r", [128, 36])
    if True:
        w_gate = din("w_gate", [NE, D, DE])
        w_up = din("w_up", [NE, D, DE])
        w_down = din("w_down", [NE, DE, D])

    y_main = dout("y_main", [TV, D])
    conv_p = dout("conv_p", [30, 1024])
    hgrn_p = dout("hgrn_p", [8, 128, 128])
    conv_s = dout("conv_s", [16, 30, 1024])
    hgrn_s = dout("hgrn_s", [16, 8, 128, 128])

    w_in_v = w_in.rearrange("(kc p) c -> p kc c", p=128)
    w_out_v = w_out.rearrange("(kc p) c -> p kc c", p=128)
    w_r_v = w_r.rearrange("(p kc) c -> p kc c", kc=NK)

    es = ExitStack()
    with es:
        k = K(nc, es)

        def sb(name, shape, dt=F32, stack=None):
            return (stack or es).enter_context(nc.sbuf_tensor(name, list(shape), dt))

        ps = [es.enter_context(nc.psum_tensor("ps%d" % i, [128, 512], F32)) for i in range(8)]
        pst = [T("ps%d" % i) for i in range(8)]

        ident_bf = sb("ident_bf", [128, 128], BF16)
        ones_f = sb("ones_f", [128, 128], F32)
        pv = sb("pv", [128, NPV], F32)
        lb = sb("lb", [128, 8], F32)
        oml = sb("oml", [128, 8], F32)
        pv_eps = sb("pv_eps", [128, 1], F32)
        wslots = [sb("wslot%d" % i, [128, NK, 512], BF16) for i in range(2)]
        cat = sb("cat", [128, NK, TV], BF16)
        catT = T()
        catTs = [T() for _ in range(NK)]
        stM = ExitStack()
        ident_f = sb("ident_f", [128, 128], F32, stM)
        maskP = sb("maskP", [128, 128], F32, stM)
        maskS = sb("maskS", [64, 64], F32, stM)
        rmask = sb("rmask", [128, TV], F32, stM)
        gxm = sb("gxm", [128, NK, 128], F32, stM)
        t_const = T("const")
        k.gconst = t_const
        S = sb("S", [128, 8, 128], F32, stM)
        S_bf = sb("S_bf", [128, 8, 128], BF16, stM)
        tS = [T("S%d" % h) for h in range(8)]
        acc = None

        def c_(fn, e="pool"):
            k.op(e, fn, writes=[t_const])

        k.dma("sp", pv[:], pvec[:, :], writes=[t_const])
        c_(lambda e: e.memset(ident_bf[:], 1.0))
        c_(lambda e: e.affine_select(out=ident_bf[:], in_=ident_bf[:], pattern=[[-1, 128]], compare_op=ALU.is_equal,
                                     fill=0.0, base=0, channel_multiplier=1))
        c_(lambda e: e.memset(ident_f[:], 1.0))
        c_(lambda e: e.affine_select(out=ident_f[:], in_=ident_f[:], pattern=[[-1, 128]], compare_op=ALU.is_equal,
                                     fill=0.0, base=0, channel_multiplier=1))
        c_(lambda e: e.memset(ones_f[:], 1.0))
        c_(lambda e: e.memset(pv_eps[:], EPS))
        c_(lambda e: e.memset(maskP[:], 1.0))
        c_(lambda e: e.affine_select(out=maskP[:], in_=maskP[:], pattern=[[1, 128]], compare_op=ALU.is_ge,
                                     fill=0.0, base=0, channel_multiplier=-1))
        c_(lambda e: e.memset(maskP[0:64, 64:128], 0.0))
        c_(lambda e: e.memset(maskS[:], 1.0))
        c_(lambda e: e.affine_select(out=maskS[:], in_=maskS[:], pattern=[[4, 16], [1, 4]], compare_op=ALU.is_ge,
                                     fill=0.0, base=0, channel_multiplier=-1))
        c_(lambda e: e.affine_select(out=maskS[:], in_=maskS[:], pattern=[[-4, 16], [0, 4]], compare_op=ALU.is_ge,
                                     fill=0.0, base=0, channel_multiplier=1))
        c_(lambda e: e.memset(rmask[:], 1.0))
        c_(lambda e: e.memset(rmask[:, 0:TCUR].rearrange("p (c j) -> p c j", j=64)[:, :, 0:1], 0.0))
        c_(lambda e: e.memset(rmask[:, TCUR:TV].rearrange("p (c j) -> p c j", j=4)[:, :, 0:1], 0.0))
        c_(lambda e: e.memset(S[:], 0.0))
        c_(lambda e: e.memset(S_bf[:], 0.0))
        c_(lambda e: e.tensor_tensor(out=lb[:], in0=pv[:, PV_L0:PV_L0 + 8], in1=pv[:, PV_L1:PV_L1 + 8], op=ALU.subtract), "dve")
        c_(lambda e: e.activation(out=lb[:], in_=lb[:], func=AF.Sigmoid), "act")
        c_(lambda e: e.tensor_scalar(out=oml[:], in0=lb[:], scalar1=-1.0, scalar2=1.0, op0=ALU.mult, op1=ALU.add), "dve")
        for kc in range(NK):
            c_(lambda e: e.tensor_scalar(out=gxm[:, kc, :], in0=ones_f[:], scalar1=pv[:, PV_GMIX + kc:PV_GMIX + kc + 1],
                                         scalar2=None, op0=ALU.mult), "dve")
        Xs = nc.dram_tensor("Xs", [NE * CAP, D], BF16, kind="Internal").ap()
        Ys = nc.dram_tensor("Ys", [NE * CAP, D], BF16, kind="Internal").ap()
        XsT = T()
        YsT = T()
        XsZ = [T() for _ in range(NE * CAP // 128)]
        for ex in range(NE * CAP // 128):
            k.dma("sp", Xs[ex * 128:(ex + 1) * 128, :], zeros_d[:, :], writes=[XsZ[ex]], bg=True)
        k.phase_end()

        def load_norm_transpose(st, xsrc, nrows_list, hT, gx, tag, sbsrc=None, hTt=None):
            if sbsrc is None:
                xt = [sb("xt%s%d" % (tag, i), [128, D], F32, st) for i in range(2)]
                xtT = [T() for _ in range(2)]
            hb = [sb("hb%s%d" % (tag, i), [128, D], BF16, st) for i in range(2)]
            hbT = [T() for _ in range(2)]
            junk = sb("junk" + tag, [128, D], BF16, st)
            junkT = T()
            ss = sb("ss" + tag, [128, 4], F32, st)
            ssT = T()
            if hTt is None:
                hTt = T()
            row = 0
            for i, nr in enumerate(nrows_list):
                b = i % 2
                if sbsrc is None:
                    k.dma("sp", xt[b][0:nr, :], xsrc[row:row + nr, :], writes=[xtT[b]])
                    xin, xinT = xt[b][0:nr, :], xtT[b]
                else:
                    xin, xinT = sbsrc[0][0:nr, i, :], sbsrc[1][i]
                k.op("act", lambda e: e.activation(out=junk[0:nr, :], in_=xin, func=AF.Square,
                                                   accum_out=ss[0:nr, 0:1]), reads=[xinT], writes=[junkT, ssT])
                k.op("act", lambda e: e.activation(out=ss[0:nr, 1:2], in_=ss[0:nr, 0:1], func=AF.Sqrt, scale=1.0 / D,
                                                   bias=pv_eps[0:nr, :]), writes=[ssT])
                k.op("dve", lambda e: e.reciprocal(out=ss[0:nr, 2:3], in_=ss[0:nr, 1:2]), writes=[ssT])
                k.op("act", lambda e: e.activation(out=hb[b][0:nr, :], in_=xin, func=AF.Copy,
                                                   scale=ss[0:nr, 2:3]), reads=[xinT, ssT], writes=[hbT[b]])
                for half in range(2):
                    pb = half
                    pbf = ps[pb][:].bitcast(BF16)

                    def tr(e):
                        ins = None
                        for j in range(8):
                            kc = half * 8 + j
                            ins = e.transpose(out=pbf[:, j * 128:j * 128 + nr], in_=hb[b][0:nr, kc * 128:(kc + 1) * 128],
                                              identity=ident_bf[0:nr, 0:nr])
                        return ins
                    k.op("pe", tr, reads=[hbT[b]], writes=[pst[pb]])
                    k.op("dve", lambda e: e.tensor_tensor(
                        out=hT[:, half * 8:half * 8 + 8, row:row + nr],
                        in0=pbf.rearrange("p (j t) -> p j t", t=128)[:, :, 0:nr],
                        in1=gx[:, half * 8:half * 8 + 8, 0:nr], op=ALU.mult), reads=[pst[pb]], writes=[hTt])
                row += nr
            return hTt


        wslotT = [T() for _ in range(2)]
        wctr = [0]

        W_SPECS = ([("in", [(4096, 512)]), ("in", [(4608, 512)]), ("in", [(3072, 512)]), ("in", [(3584, 512)])]
                   + [("in", [(cc * 128, 128), (1024 + cc * 128, 128)]) for cc in range(8)]
                   + [("in", [(4096, 512)]), ("in", [(4608, 512)])]
                   + [("in", [(3072 + h * 128, 128), (2048 + h * 128, 128), (5120 + h * 128, 128)]) for h in range(8)]
                   + [("out", [(cb * 512, 512)]) for cb in range(4)])
        wissued = [0]
        wg_bf = nc.dram_tensor("wg_bf", [NE, 128, NK, DE], BF16, kind="Internal").ap()
        wu_bf = nc.dram_tensor("wu_bf", [NE, 128, NK, DE], BF16, kind="Internal").ap()
        wd_bf = nc.dram_tensor("wd_bf", [NE, 128, 4, D], BF16, kind="Internal").ap()
        pcT = [[T(), T(), T()] for _ in range(NE)]
        pcq = [(ex, j) for ex in range(NPRE) for j in range(3)]
        pci = [0]

        def precast(n):
            for _ in range(n):
                if pci[0] >= len(pcq):
                    return
                ex, j = pcq[pci[0]]
                pci[0] += 1
                srcw, dstw = ((w_gate, wg_bf), (w_up, wu_bf), (w_down, wd_bf))[j]
                if j < 2:
                    sv = srcw[ex].rearrange("(p kc) f -> p kc f", kc=NK)
                else:
                    sv = srcw[ex].rearrange("(kc p) f -> p kc f", p=128)
                if j < 2:
                    k.dma("pool", dstw[ex], sv, writes=[pcT[ex][j]], bg=True, max_dma_last_dim=4096)
                else:
                    k.dma("pool", dstw[ex], sv, writes=[pcT[ex][j]], bg=True)

        def _issue_w(j):
            which, col_ranges = W_SPECS[j]
            src_view = w_in_v if which == "in" else w_out_v
            s = j % 2
            off = 0
            for (c0, n) in col_ranges:
                k.dma("pool", wslots[s][:, :, off:off + n], src_view[:, :, c0:c0 + n], writes=[wslotT[s]])
                off += n

        def load_w(src_view, col_ranges, ahead=1):
            j = wctr[0]
            wctr[0] += 1
            assert W_SPECS[j][1] == col_ranges, (j, W_SPECS[j], col_ranges)
            while wissued[0] <= min(j + ahead, len(W_SPECS) - 1):
                _issue_w(wissued[0])
                precast(4)
                wissued[0] += 1
            return wslots[j % 2], wslotT[j % 2]

        def gates_f(st_, pf, pfT, ncol, lbh, omlh, rm, outs):
            pass

        with ExitStack() as st:
            hTp = sb("hTp", [128, NK, TPREV], BF16, st)
            hTpT = load_norm_transpose(st, x_prev, [128] * 8, hTp, gxm, "p")
            ktok = sb("ktokp", [128, 8, 8, 128], BF16, st)
            ktokT = T()
            vtok = sb("vtokp", [128, 8, 1024], BF16, st)
            vtokT = T()
            ebl = sb("eblp", [128, 8, 16], F32, st)
            eblT = T()
            tmpfA = [[sb("tmpfA%d_%d" % (q_, i), [128, 512], F32, st) for i in range(4)] for q_ in range(2)]
            tmpTA = [[T() for _ in range(4)] for q_ in range(2)]
            khatA = [sb("khatA%d" % q_, [128, 512], BF16, st) for q_ in range(2)]
            khatTA = [T() for q_ in range(2)]
            for blk in range(2):
                wsl, wT = load_w(w_in_v, [(4096 + blk * 512, 512)])
                for tt in range(8):
                    pb = 2 + (tt % 2)

                    def mmv(e):
                        ins = None
                        for kc in range(NK):
                            ins = e.matmul(ps[pb][:], lhsT=hTp[:, kc, tt * 128:(tt + 1) * 128], rhs=wsl[:, kc, :],
                                           start=(kc == 0), stop=(kc == NK - 1))
                        return ins
                    k.op("pe", mmv, reads=[hTpT, wT], writes=[pst[pb]])
                    k.op("act", lambda e: e.copy(out=vtok[:, tt, blk * 512:(blk + 1) * 512], in_=ps[pb][:]),
                         reads=[pst[pb]], writes=[vtokT])
            for hb_ in range(2):
                wsl, wT = load_w(w_in_v, [(3072 + hb_ * 512, 512)])
                for hh in range(4):
                    h = hb_ * 4 + hh
                    for nt in range(2):
                        pb = 4 + (nt % 2)

                        def mmf(e):
                            ins = None
                            for kc in range(NK):
                                ins = e.matmul(ps[pb][:], lhsT=wsl[:, kc, hh * 128:(hh + 1) * 128],
                                               rhs=hTp[:, kc, nt * 512:(nt + 1) * 512], start=(kc == 0), stop=(kc == NK - 1))
                            return ins
                        k.op("pe", mmf, reads=[hTpT, wT], writes=[pst[pb]])
                        tmpf, tmpT, khat, khatT = tmpfA[nt % 2], tmpTA[nt % 2], khatA[nt % 2], khatTA[nt % 2]
                        fs, fg, bc, em = tmpf
                        k.op("act", lambda e: e.activation(out=fs[:], in_=ps[pb][:], func=AF.Sigmoid),
                             reads=[pst[pb]], writes=[tmpT[0]])
                        k.op("dve", lambda e: e.tensor_scalar(out=fg[:], in0=fs[:], scalar1=oml[:, h:h + 1],
                                                              scalar2=lb[:, h:h + 1], op0=ALU.mult, op1=ALU.add),
                             reads=[tmpT[0]], writes=[tmpT[1]])
                        k.op("act", lambda e: e.activation(out=fs[:], in_=fg[:], func=AF.Ln),
                             reads=[tmpT[1]], writes=[tmpT[0]])
                        k.op("dve", lambda e: e.tensor_tensor_scan(out=bc[:], data0=rmask[:, 0:512], data1=fs[:],
                                                                   initial=0.0, op0=ALU.mult, op1=ALU.add),
                             reads=[tmpT[0]], writes=[tmpT[2]])
                        k.op("act", lambda e: e.activation(out=em[:], in_=bc[:], func=AF.Exp, scale=-1.0),
                             reads=[tmpT[2]], writes=[tmpT[3]])
                        k.op("act", lambda e: e.activation(
                            out=ebl[:, h, nt * 8:(nt + 1) * 8],
                            in_=bc[:].rearrange("p (c j) -> p c j", j=64)[:, :, 63], func=AF.Exp),
                            reads=[tmpT[2]], writes=[eblT])
                        k.op("dve", lambda e: e.tensor_tensor(out=fs[:], in0=fg[:], in1=em[:], op=ALU.mult),
                             reads=[tmpT[1], tmpT[3]], writes=[tmpT[0]])
                        k.op("dve", lambda e: e.tensor_tensor(out=khat[:], in0=em[:], in1=fs[:], op=ALU.subtract),
                             reads=[tmpT[0], tmpT[3]], writes=[khatT])
                        pbt = 6 + (nt % 2)
                        pbf = ps[pbt][:].bitcast(BF16)

                        def trk(e):
                            ins = None
                            for j in range(4):
                                ins = e.transpose(out=pbf[:, j * 128:(j + 1) * 128], in_=khat[:, j * 128:(j + 1) * 128],
                                                  identity=ident_bf[:])
                            return ins
                        k.op("pe", trk, reads=[khatT], writes=[pst[pbt]])
                        k.op("act", lambda e: e.copy(out=ktok[:, nt * 4:(nt + 1) * 4, h, :],
                                                     in_=pbf[:, 0:512].rearrange("p (j d) -> p j d", d=128)),
                             reads=[pst[pbt]], writes=[ktokT])
            stmp = [sb("stmpA%d" % i, [128, 128], F32, st) for i in range(2)]
            stmpT = [T() for _ in range(2)]
            for c in range(16):
                tt, r0 = c // 2, (c % 2) * 64
                for h in range(8):
                    pb = h % 4
                    k.op("pe", lambda e: e.matmul(ps[pb][:, 0:128], lhsT=ktok[r0:r0 + 64, tt, h, :],
                                                  rhs=vtok[r0:r0 + 64, tt, h * 128:(h + 1) * 128], start=True, stop=True),
                         reads=[ktokT, vtokT], writes=[pst[pb]])
                    b2 = h % 2
                    k.op("dve", lambda e: e.tensor_tensor(out=stmp[b2][:], in0=ps[pb][:, 0:128], in1=S[:, h, :], op=ALU.add),
                         reads=[pst[pb], tS[h]], writes=[stmpT[b2]])
                    k.op("dve", lambda e: e.tensor_scalar(out=S[:, h, :], in0=stmp[b2][:], scalar1=ebl[:, h, c:c + 1],
                                                          scalar2=None, op0=ALU.mult),
                         reads=[stmpT[b2], eblT], writes=[tS[h]])
            for h in range(8):
                k.op("act", lambda e: e.copy(out=S_bf[:, h, :], in_=S[:, h, :]), writes=[tS[h]])
            k.phase_end()

        if stage < 2:
            for h in range(8):
                k.dma("sp", hgrn_p[h, :, :], S[:, h, :], reads=[tS[h]])
            k.drain_dmas("sp")
            k.nc.all_engine_barrier()
            return nc

        TT = [(i * 128, 128) for i in range(8)] + [(1024, 64)]
        NT = [(0, 512), (512, 512), (1024, 64)]
        wdw = pv[:, PV_WDW:PV_WDW + 8 * CONVW]
        with ExitStack() as st:
            hT = sb("hT", [128, NK, TM], BF16, st)
            with ExitStack() as stt:
                hTt = load_norm_transpose(stt, x_main, [128] * 8 + [96], hT, gxm, "m")
                k.phase_end()
            with ExitStack() as s1:
                extc = [sb("extc%d" % i, [128, 1056], F32, s1) for i in range(2)]
                extSc = [sb("extSc%d" % i, [128, 16, 34], F32, s1) for i in range(2)]
                tails = sb("tails", [128, 8, 32], F32, s1)
                tailsT = T()
                us = sb("us", [128, 8, 64], F32, s1)
                usT = T()
                cv = sb("cv", [128, 8, TV], F32, s1)
                extT = [T() for _ in range(2)]
                extST = [T() for _ in range(2)]
                cvT = [T() for _ in range(8)]
                sct = [sb("sct%d" % i, [120, 1024], F32, s1) for i in range(4)]
                sctT = [T() for _ in range(4)]
                k.dma("sp", conv_s[:, 0:26, :], sconv[:, 4:30, :])
                for g4 in range(4):
                    k.dma("sp", sct[g4][:], sconv[g4 * 4:(g4 + 1) * 4].rearrange("b j c -> (b j) c"), writes=[sctT[g4]])
                sg = [sb("sgB%d" % i, [128, 512], F32, s1) for i in range(2)]
                sgT = [T() for _ in range(2)]
                for cc in range(8):
                    c2 = cc % 2
                    for g4 in range(4):
                        pb = g4 % 2
                        k.op("pe", lambda e: e.transpose(out=ps[pb][:, 0:120], in_=sct[g4][:, cc * 128:(cc + 1) * 128],
                                                         identity=ident_f[0:120, 0:120]), reads=[sctT[g4]], writes=[pst[pb]])
                        k.op("act", lambda e: e.copy(out=extSc[c2][:, g4 * 4:(g4 + 1) * 4, 0:30],
                                                     in_=ps[pb][:, 0:120].rearrange("p (b j) -> p b j", j=30)),
                             reads=[pst[pb]], writes=[extST[c2]])
                    wsl, wT = load_w(w_in_v, [(cc * 128, 128), (1024 + cc * 128, 128)])
                    for nt, (t0, n) in enumerate([(0, 512), (512, 512), (1024, 96)]):
                        pa = 2 + (nt % 2) * 2
                        pg = pa + 1

                        def mma(e):
                            ins = None
                            for kc in range(NK):
                                ins = e.matmul(ps[pa][:, 0:n], lhsT=wsl[:, kc, 0:128], rhs=hT[:, kc, t0:t0 + n],
                                               start=(kc == 0), stop=(kc == NK - 1))
                            return ins

                        def mmg(e):
                            ins = None
                            for kc in range(NK):
                                ins = e.matmul(ps[pg][:, 0:n], lhsT=wsl[:, kc, 128:256], rhs=hT[:, kc, t0:t0 + n],
                                               start=(kc == 0), stop=(kc == NK - 1))
                            return ins
                        k.op("pe", mma, reads=[hTt, wT], writes=[pst[pa]])
                        k.op("pe", mmg, reads=[hTt, wT], writes=[pst[pg]])
                        s_ = nt % 2
                        k.op("act", lambda e: e.activation(out=sg[s_][:, 0:n], in_=ps[pg][:, 0:n], func=AF.Sigmoid),
                             reads=[pst[pg]], writes=[sgT[s_]])
                        if nt < 2:
                            k.op("dve", lambda e: e.tensor_tensor(out=extc[c2][:, 32 + t0:32 + t0 + n], in0=ps[pa][:, 0:n],
                                                                  in1=sg[s_][:, 0:n], op=ALU.mult),
                                 reads=[pst[pa], sgT[s_]], writes=[extT[c2]])
                        else:
                            k.op("dve", lambda e: e.tensor_tensor(
                                out=extSc[c2][:, :, 30:34], in0=ps[pa][:, 0:64].rearrange("p (b t) -> p b t", t=4),
                                in1=sg[s_][:, 0:64].rearrange("p (b t) -> p b t", t=4), op=ALU.mult),
                                reads=[pst[pa], sgT[s_]], writes=[extST[c2]])
                            k.op("dve", lambda e: e.tensor_tensor(out=extc[c2][:, 0:32], in0=ps[pa][:, 64:96],
                                                                  in1=sg[s_][:, 64:96], op=ALU.mult),
                                 reads=[pst[pa], sgT[s_]], writes=[extT[c2]])
                    k.op("dve", lambda e: e.tensor_scalar(out=cv[:, cc, 0:TCUR], in0=extc[c2][:, 2:2 + TCUR],
                                                          scalar1=wdw[:, cc * CONVW:cc * CONVW + 1],
                                                          scalar2=pv[:, PV_BDW + cc:PV_BDW + cc + 1], op0=ALU.mult, op1=ALU.add),
                         reads=[extT[c2]], writes=[cvT[cc]])
                    for j in range(1, CONVW):
                        k.op("dve", lambda e: e.scalar_tensor_tensor(out=cv[:, cc, 0:TCUR], in0=extc[c2][:, 2 + j:2 + j + TCUR],
                                                                     scalar=wdw[:, cc * CONVW + j:cc * CONVW + j + 1],
                                                                     in1=cv[:, cc, 0:TCUR], op0=ALU.mult, op1=ALU.add),
                             reads=[extT[c2]], writes=[cvT[cc]])
                    cvs = cv[:, cc, TCUR:TV].rearrange("p (b t) -> p b t", t=4)
                    k.op("dve", lambda e: e.tensor_scalar(out=cvs, in0=extSc[c2][:, :, 0:4],
                                                          scalar1=wdw[:, cc * CONVW:cc * CONVW + 1],
                                                          scalar2=pv[:, PV_BDW + cc:PV_BDW + cc + 1], op0=ALU.mult, op1=ALU.add),
                         reads=[extST[c2]], writes=[cvT[cc]])
                    for j in range(1, CONVW):
                        k.op("dve", lambda e: e.scalar_tensor_tensor(out=cvs, in0=extSc[c2][:, :, j:j + 4],
                                                                     scalar=wdw[:, cc * CONVW + j:cc * CONVW + j + 1],
                                                                     in1=cvs, op0=ALU.mult, op1=ALU.add),
                             reads=[extST[c2]], writes=[cvT[cc]])
                    k.op("act", lambda e: e.copy(out=tails[:, cc, :], in_=extc[c2][:, 1024:1056]), reads=[extT[c2]], writes=[tailsT])
                    k.op("act", lambda e: e.copy(out=us[:, cc, :].rearrange("p (b t) -> p b t", t=4), in_=extSc[c2][:, :, 30:34]),
                         reads=[extST[c2]], writes=[usT])
                cpo = sb("cpo", [64, 1024], F32, s1)
                cpoT = T()
                for half in range(2):
                    pb = half

                    def trc(e):
                        ins = None
                        for j in range(4):
                            cc = half * 4 + j
                            ins = e.transpose(out=ps[pb][0:32, j * 128:(j + 1) * 128], in_=tails[:, cc, :],
                                              identity=ident_f[:])
                        return ins
                    k.op("pe", trc, reads=[tailsT], writes=[pst[pb]])
                    k.op("act", lambda e: e.copy(out=cpo[0:32, half * 512:(half + 1) * 512], in_=ps[pb][0:32, :]),
                         reads=[pst[pb]], writes=[cpoT])
                k.dma("sp", conv_p[:, :], cpo[2:32, :], reads=[cpoT])
                cso = cpo
                csoT = cpoT
                for half in range(2):
                    pb = 2 + half

                    def trs(e):
                        ins = None
                        for j in range(4):
                            cc = half * 4 + j
                            ins = e.transpose(out=ps[pb][0:64, j * 128:(j + 1) * 128], in_=us[:, cc, :], identity=ident_f[:])
                        return ins
                    k.op("pe", trs, reads=[usT], writes=[pst[pb]])
                    k.op("act", lambda e: e.copy(out=cso[:, half * 512:(half + 1) * 512], in_=ps[pb][0:64, :]),
                         reads=[pst[pb]], writes=[csoT])
                for b in range(16):
                    k.dma("sp", conv_s[b, 26:30, :], cso[b * 4:(b + 1) * 4, :], reads=[csoT])
                sq = [sb("sqB%d" % i, [128, 512], F32, s1) for i in range(2)]
                sqT = [T() for _ in range(2)]
                mean = sb("meanB", [128, 512], F32, s1)
                rstd = sb("rstdB", [128, 512], F32, s1)
                msq = sb("msqB", [128, 512], F32, s1)
                stT = T()
                t1 = sg
                t1T = sgT
                for (t0, n) in NT:
                    for cc in range(8):
                        s_ = cc % 2
                        k.op("act", lambda e: e.activation(out=sq[s_][:, 0:n], in_=cv[:, cc, t0:t0 + n], func=AF.Square),
                             reads=[cvT[cc]], writes=[sqT[s_]])
                        k.op("pe", lambda e: e.matmul(ps[6][:, 0:n], lhsT=ones_f[:], rhs=cv[:, cc, t0:t0 + n],
                                                      start=(cc == 0), stop=(cc == 7)), reads=[cvT[cc]], writes=[pst[6]])
                        k.op("pe", lambda e: e.matmul(ps[7][:, 0:n], lhsT=ones_f[:], rhs=sq[s_][:, 0:n],
                                                      start=(cc == 0), stop=(cc == 7)), reads=[sqT[s_]], writes=[pst[7]])
                    k.op("dve", lambda e: e.tensor_scalar(out=mean[:, 0:n], in0=ps[6][:, 0:n], scalar1=1.0 / 1024, scalar2=None,
                                                          op0=ALU.mult), reads=[pst[6]], writes=[stT])
                    k.op("dve", lambda e: e.tensor_tensor(out=msq[:, 0:n], in0=mean[:, 0:n], in1=mean[:, 0:n], op=ALU.mult),
                         writes=[stT])
                    k.op("dve", lambda e: e.scalar_tensor_tensor(out=msq[:, 0:n], in0=ps[7][:, 0:n], scalar=1.0 / 1024,
                                                                 in1=msq[:, 0:n], op0=ALU.mult, op1=ALU.subtract),
                         reads=[pst[7]], writes=[stT])
                    k.op("act", lambda e: e.activation(out=msq[:, 0:n], in_=msq[:, 0:n], func=AF.Sqrt, bias=pv_eps[:, :]),
                         writes=[stT])
                    k.op("dve", lambda e: e.reciprocal(out=rstd[:, 0:n], in_=msq[:, 0:n]), writes=[stT])
                    for cc in range(8):
                        s_ = cc % 2
                        k.op("dve", lambda e: e.tensor_tensor(out=t1[s_][:, 0:n], in0=cv[:, cc, t0:t0 + n], in1=mean[:, 0:n],
                                                              op=ALU.subtract), reads=[cvT[cc], stT], writes=[t1T[s_]])
                        k.op("dve", lambda e: e.tensor_tensor(out=t1[s_][:, 0:n], in0=t1[s_][:, 0:n], in1=rstd[:, 0:n],
                                                              op=ALU.mult), reads=[stT], writes=[t1T[s_]])
                        k.op("act", lambda e: e.activation(out=cat[:, cc, t0:t0 + n], in_=t1[s_][:, 0:n], func=AF.Silu,
                                                           scale=pv[:, PV_LNG + cc:PV_LNG + cc + 1],
                                                           bias=pv[:, PV_LNB + cc:PV_LNB + cc + 1]),
                             reads=[t1T[s_]], writes=[catTs[cc]])
                k.phase_end()
            if stage < 3:
                es.pop_all()
                return nc
            with ExitStack() as s2:
                vtok = sb("vtok", [128, 9, 1024], BF16, s2)
                vtokT = T()
                for blk in range(2):
                    wsl, wT = load_w(w_in_v, [(4096 + blk * 512, 512)])
                    for tt, (r0, nr) in enumerate(TT):
                        pb = tt % 2

                        def mmv(e):
                            ins = None
                            for kc in range(NK):
                                ins = e.matmul(ps[pb][0:nr, :], lhsT=hT[:, kc, r0:r0 + nr], rhs=wsl[:, kc, :],
                                               start=(kc == 0), stop=(kc == NK - 1))
                            return ins
                        k.op("pe", mmv, reads=[hTt, wT], writes=[pst[pb]])
                        k.op("act", lambda e: e.copy(out=vtok[0:nr, tt, blk * 512:(blk + 1) * 512], in_=ps[pb][0:nr, :]),
                             reads=[pst[pb]], writes=[vtokT])
                qtS = sb("qtS", [128, 8, 64], BF16, s2)
                sggS = sb("sggS", [128, 8, 64], BF16, s2)
                attmS = sb("attmS", [64, 8, 64], BF16, s2)
                qtST, sggST, attmST = T(), T(), T()
                zer = sb("zer", [128, 128], BF16, s2)
                ind = sb("ind", [64, 16], F32, s2)
                indT = T()
                k.op("pool", lambda e: e.memset(zer[:], 0.0), writes=[indT])
                k.op("pool", lambda e: e.memset(ind[:], 1.0), writes=[indT])
                k.op("pool", lambda e: e.affine_select(out=ind[:], in_=ind[:], pattern=[[-4, 16]], compare_op=ALU.is_ge,
                                                       fill=0.0, base=0, channel_multiplier=1), writes=[indT])
                k.op("pool", lambda e: e.affine_select(out=ind[:], in_=ind[:], pattern=[[4, 16]], compare_op=ALU.is_ge,
                                                       fill=0.0, base=3, channel_multiplier=-1), writes=[indT])
                ktokS = sb("ktokS", [64, 8, 128], BF16, s2)
                ktokST = T()
                ebS = sb("ebS", [128, 8, 16], F32, s2)
                ebST = T()
                gn = pv[:, PV_GN:PV_GN + 1]
                sH = ExitStack()
                R2 = []
                for p in range(2):
                    r = {}
                    r["khat"] = sb("khatB%d" % p, [128, TV], BF16, sH)
                    r["qt"] = sb("qtB%d" % p, [128, TV], BF16, sH)
                    r["sgg"] = sb("sggB%d" % p, [128, TV], BF16, sH)
                    r["ktok"] = sb("ktokB%d" % p, [128, 8, 128], BF16, sH)
                    r["ebl"] = sb("eblB%d" % p, [128, 16], F32, sH)
                    r["tmpf"] = [sb("tmpfB%d_%d" % (p, i), [128, 512], F32, sH) for i in range(5)]
                    r["attm"] = sb("attm%d" % p, [128, 128], BF16, sH)
                    r["osq"] = sb("osq%d" % p, [128, 128], F32, sH)
                    r["orr"] = sb("orr%d" % p, [128, 128], F32, sH)
                    r["stmp"] = sb("stmpB%d" % p, [128, 128], F32, sH)
                    for nm in ("khatT", "qtT", "sggT", "ktokT", "eblT", "attmT", "osqT", "orrT", "stmpT"):
                        r[nm] = T()
                    r["tmpT"] = [T() for _ in range(5)]
                    R2.append(r)

                def head_gen(h):
                    p = h % 2
                    r = R2[p]
                    khat, qt, sgg, ktok, ebl, tmpf, attm, osq, orr, stmp = (r["khat"], r["qt"], r["sgg"], r["ktok"], r["ebl"],
                                                                           r["tmpf"], r["attm"], r["osq"], r["orr"], r["stmp"])
                    khatT, qtT, sggT, ktokT, eblT, attmT, osqT, orrT, stmpT, tmpT = (r["khatT"], r["qtT"], r["sggT"], r["ktokT"],
                                                                                     r["eblT"], r["attmT"], r["osqT"], r["orrT"],
                                                                                     r["stmpT"], r["tmpT"])
                    B0 = 4 * p
                    wsl, wT = load_w(w_in_v, [(3072 + h * 128, 128), (2048 + h * 128, 128), (5120 + h * 128, 128)], ahead=0)
                    for nt, (t0, n) in enumerate(NT):
                        for ci in range(3):
                            pb = B0 + ci

                            def mmx(e):
                                ins = None
                                for kc in range(NK):
                                    ins = e.matmul(ps[pb][:, 0:n], lhsT=wsl[:, kc, ci * 128:(ci + 1) * 128],
                                                   rhs=hT[:, kc, t0:t0 + n], start=(kc == 0), stop=(kc == NK - 1))
                                return ins
                            k.op("pe", mmx, reads=[hTt, wT], writes=[pst[pb]])
                        yield
                        fs, fg, bc, em, qs = tmpf
                        k.op("act", lambda e: e.activation(out=fs[:, 0:n], in_=ps[B0][:, 0:n], func=AF.Sigmoid),
                             reads=[pst[B0]], writes=[tmpT[0]])
                        k.op("act", lambda e: e.activation(out=qs[:, 0:n], in_=ps[B0 + 1][:, 0:n], func=AF.Silu),
                             reads=[pst[B0 + 1]], writes=[tmpT[4]])
                        k.op("act", lambda e: e.activation(out=sgg[:, t0:t0 + n], in_=ps[B0 + 2][:, 0:n], func=AF.Silu),
                             reads=[pst[B0 + 2]], writes=[sggT])
                        k.op("dve", lambda e: e.tensor_scalar(out=fg[:, 0:n], in0=fs[:, 0:n], scalar1=oml[:, h:h + 1],
                                                              scalar2=lb[:, h:h + 1], op0=ALU.mult, op1=ALU.add),
                             reads=[tmpT[0]], writes=[tmpT[1]])
                        k.op("act", lambda e: e.activation(out=fs[:, 0:n], in_=fg[:, 0:n], func=AF.Ln),
                             reads=[tmpT[1]], writes=[tmpT[0]])
                        k.op("dve", lambda e: e.tensor_tensor_scan(out=bc[:, 0:n], data0=rmask[:, t0:t0 + n], data1=fs[:, 0:n],
                                                                   initial=0.0, op0=ALU.mult, op1=ALU.add),
                             reads=[tmpT[0]], writes=[tmpT[2]])
                        yield
                        k.op("act", lambda e: e.activation(out=em[:, 0:n], in_=bc[:, 0:n], func=AF.Exp, scale=-1.0),
                             reads=[tmpT[2]], writes=[tmpT[3]])
                        k.op("act", lambda e: e.activation(out=fs[:, 0:n], in_=bc[:, 0:n], func=AF.Exp),
                             reads=[tmpT[2]], writes=[tmpT[0]])
                        if nt < 2:
                            k.op("act", lambda e: e.copy(out=ebl[:, nt * 8:(nt + 1) * 8],
                                                         in_=fs[:, 0:n].rearrange("p (c j) -> p c j", j=64)[:, :, 63]),
                                 reads=[tmpT[0]], writes=[eblT])
                        else:
                            k.op("act", lambda e: e.copy(out=ebS[:, h, :],
                                                         in_=fs[:, 0:n].rearrange("p (c j) -> p c j", j=4)[:, :, 3]),
                                 reads=[tmpT[0]], writes=[ebST])
                        k.op("dve", lambda e: e.tensor_tensor(out=qt[:, t0:t0 + n], in0=qs[:, 0:n], in1=fs[:, 0:n], op=ALU.mult),
                             reads=[tmpT[4], tmpT[0]], writes=[qtT])
                        k.op("dve", lambda e: e.tensor_tensor(out=fg[:, 0:n], in0=fg[:, 0:n], in1=em[:, 0:n], op=ALU.mult),
                             reads=[tmpT[3]], writes=[tmpT[1]])
                        k.op("dve", lambda e: e.tensor_tensor(out=khat[:, t0:t0 + n], in0=em[:, 0:n], in1=fg[:, 0:n],
                                                              op=ALU.subtract), reads=[tmpT[1], tmpT[3]], writes=[khatT])
                        yield
                    pbt = B0 + 3
                    pbf = ps[pbt][:].bitcast(BF16)
                    for g2 in range(2):
                        def trk(e):
                            ins = None
                            for j in range(4):
                                tt = g2 * 4 + j
                                ins = e.transpose(out=pbf[:, j * 128:(j + 1) * 128], in_=khat[:, tt * 128:(tt + 1) * 128],
                                                  identity=ident_bf[:])
                            return ins
                        k.op("pe", trk, reads=[khatT], writes=[pst[pbt]])
                        k.op("act", lambda e: e.copy(out=ktok[:, g2 * 4:(g2 + 1) * 4, :],
                                                     in_=pbf[:, 0:512].rearrange("p (j d) -> p j d", d=128)),
                             reads=[pst[pbt]], writes=[ktokT])
                    k.op("pe", lambda e: e.transpose(out=pbf[0:64, 0:128], in_=khat[:, 1024:1088], identity=ident_bf[:]),
                         reads=[khatT], writes=[pst[pbt]])
                    k.op("act", lambda e: e.copy(out=ktokS[:, h, :], in_=pbf[0:64, 0:128]), reads=[pst[pbt]], writes=[ktokST])
                    yield
                    pA, pO, pS, pQ = B0, B0 + 1, B0 + 2, B0 + 3
                    for pp in range(8):
                        c0 = pp * 128
                        k.op("pe", lambda e: e.matmul(ps[pA][:, 0:128], lhsT=khat[:, c0:c0 + 128], rhs=qt[:, c0:c0 + 128],
                                                      start=True, stop=True), reads=[khatT, qtT], writes=[pst[pA]])
                        k.op("dve", lambda e: e.tensor_tensor(out=attm[:], in0=ps[pA][:, 0:128], in1=maskP[:], op=ALU.mult),
                             reads=[pst[pA]], writes=[attmT])
                        k.op("pe", lambda e: e.matmul(ps[pO][:, 0:128], lhsT=vtok[:, pp, h * 128:(h + 1) * 128], rhs=attm[:],
                                                      start=True, stop=False), reads=[vtokT, attmT], writes=[pst[pO]])
                        yield
                        for sub in range(2):
                            cs = c0 + sub * 64
                            r0 = sub * 64
                            k.op("pe", lambda e: e.matmul(ps[pO][:, r0:r0 + 64], lhsT=S_bf[:, h, :], rhs=qt[:, cs:cs + 64],
                                                          start=False, stop=(sub == 1)), reads=[tS[h], qtT], writes=[pst[pO]])
                            k.op("pe", lambda e: e.matmul(ps[pS][:, 0:128], lhsT=ktok[r0:r0 + 64, pp, :],
                                                          rhs=vtok[r0:r0 + 64, pp, h * 128:(h + 1) * 128], start=True, stop=True),
                                 reads=[ktokT, vtokT], writes=[pst[pS]])
                            k.op("dve", lambda e: e.tensor_tensor(out=stmp[:], in0=ps[pS][:, 0:128], in1=S[:, h, :], op=ALU.add),
                                 reads=[pst[pS], tS[h]], writes=[stmpT])
                            ci_ = pp * 2 + sub
                            k.op("dve", lambda e: e.tensor_scalar(out=S[:, h, :], in0=stmp[:], scalar1=ebl[:, ci_:ci_ + 1],
                                                                  scalar2=None, op0=ALU.mult),
                                 reads=[stmpT, eblT], writes=[tS[h]])
                            k.op("act", lambda e: e.copy(out=S_bf[:, h, :], in_=S[:, h, :]), writes=[tS[h]])
                            yield
                        n = 128
                        k.op("act", lambda e: e.activation(out=osq[:, 0:n], in_=ps[pO][:, 0:n], func=AF.Square),
                             reads=[pst[pO]], writes=[osqT])
                        k.op("pe", lambda e: e.matmul(ps[pQ][:, 0:n], lhsT=ones_f[:], rhs=osq[:, 0:n], start=True, stop=True),
                             reads=[osqT], writes=[pst[pQ]])
                        k.op("act", lambda e: e.activation(out=orr[:, 0:n], in_=ps[pQ][:, 0:n], func=AF.Sqrt, scale=1.0 / 128,
                                                           bias=pv_eps[:, :]), reads=[pst[pQ]], writes=[orrT])
                        yield
                        k.op("dve", lambda e: e.reciprocal(out=orr[:, 0:n], in_=orr[:, 0:n]), writes=[orrT])
                        k.op("dve", lambda e: e.tensor_tensor(out=orr[:, 0:n], in0=ps[pO][:, 0:n], in1=orr[:, 0:n], op=ALU.mult),
                             reads=[pst[pO]], writes=[orrT])
                        k.op("dve", lambda e: e.scalar_tensor_tensor(out=cat[:, 8 + h, c0:c0 + n], in0=orr[:, 0:n], scalar=gn,
                                                                     in1=sgg[:, c0:c0 + n], op0=ALU.mult, op1=ALU.mult),
                             reads=[orrT, sggT], writes=[catTs[8 + h]])
                        yield
                    k.op("pe", lambda e: e.matmul(ps[pA][0:64, 0:64], lhsT=khat[:, 1024:1088], rhs=qt[:, 1024:1088],
                                                  start=True, stop=True), reads=[khatT, qtT], writes=[pst[pA]])
                    k.op("dve", lambda e: e.tensor_tensor(out=attmS[:, h, :], in0=ps[pA][0:64, 0:64], in1=maskS[:], op=ALU.mult),
                         reads=[pst[pA]], writes=[attmST])
                    k.op("act", lambda e: e.copy(out=qtS[:, h, :], in_=qt[:, 1024:1088]), reads=[qtT], writes=[qtST])
                    k.op("act", lambda e: e.copy(out=sggS[:, h, :], in_=sgg[:, 1024:1088]), reads=[sggT], writes=[sggST])

                gens = [head_gen(h) for h in range(8)]
                active = [gens[0], gens[1]]
                nxt = 2
                for _ in range(6):
                    next(active[0])
                while active:
                    for g in list(active):
                        try:
                            next(g)
                        except StopIteration:
                            active.remove(g)
                            if nxt < 8:
                                active.append(gens[nxt])
                                nxt += 1
                k.soft_barrier()
                sH.close()
                k.dma("sp", hgrn_p.rearrange("h d v -> d h v"), S[:], reads=tS)
                def mm0(e):
                    e.matmul(ps[5][:, :], lhsT=zer[:], rhs=hT[:, 0, 0:512], start=True, stop=False)
                    ins = None
                    for h in range(8):
                        ins = e.matmul(ps[5][:, h * 64:(h + 1) * 64], lhsT=vtok[0:64, 8, h * 128:(h + 1) * 128],
                                       rhs=attmS[:, h, :], start=False, stop=False)
                    return ins
                k.op("pe", mm0, reads=[indT, vtokT, attmST, hTt], writes=[pst[5]])
                Sf = [sb("Sf%d" % i, [128, 8, 128], F32, s2) for i in range(2)]
                SfT = [T() for _ in range(2)]
                Sbb = [sb("Sbb%d" % i, [128, 8, 128], BF16, s2) for i in range(2)]
                SbbT = [T() for _ in range(2)]
                vm = [sb("vm%d" % i, [64, 1024], BF16, s2) for i in range(2)]
                vmT = [T() for _ in range(2)]
                stm2 = sb("stm2", [128, 8, 128], F32, s2)
                stm2T = T()
                for b in range(16):
                    b2 = b % 2
                    k.dma("sp", Sf[b2][:], shgrn[b].rearrange("h d v -> d h v"), writes=[SfT[b2]])
                    k.dma("pool", Sbb[b2][:], shgrn[b].rearrange("h d v -> d h v"), writes=[SbbT[b2]])

                    def mmi(e):
                        ins = None
                        for h in range(8):
                            ins = e.matmul(ps[5][:, h * 64 + 4 * b:h * 64 + 4 * b + 4], lhsT=Sbb[b2][:, h, :],
                                           rhs=qtS[:, h, 4 * b:4 * b + 4], start=False, stop=(b == 15 and h == 7))
                        return ins
                    k.op("pe", mmi, reads=[SbbT[b2], qtST], writes=[pst[5]])
                    k.op("dve", lambda e: e.tensor_scalar(out=vm[b2][:], in0=vtok[0:64, 8, :], scalar1=ind[:, b:b + 1],
                                                          scalar2=None, op0=ALU.mult), reads=[vtokT, indT], writes=[vmT[b2]])
                    for half in range(2):
                        pb = 2 + half

                        def mmu(e):
                            ins = None
                            for j in range(4):
                                h = half * 4 + j
                                ins = e.matmul(ps[pb][:, j * 128:(j + 1) * 128], lhsT=ktokS[:, h, :],
                                               rhs=vm[b2][:, h * 128:(h + 1) * 128], start=True, stop=True)
                            return ins
                        k.op("pe", mmu, reads=[ktokST, vmT[b2]], writes=[pst[pb]])
                        k.op("dve", lambda e: e.tensor_tensor(out=stm2[:, half * 4:half * 4 + 4, :],
                                                              in0=ps[pb][:].rearrange("p (j v) -> p j v", v=128),
                                                              in1=Sf[b2][:, half * 4:half * 4 + 4, :], op=ALU.add),
                             reads=[pst[pb], SfT[b2]], writes=[stm2T])
                    k.op("dve", lambda e: e.tensor_tensor(out=Sf[b2][:], in0=stm2[:],
                                                          in1=ebS[:, :, b:b + 1].to_broadcast([128, 8, 128]), op=ALU.mult),
                         reads=[stm2T, ebST], writes=[SfT[b2]])
                    k.dma("sp", hgrn_s[b].rearrange("h d v -> d h v"), Sf[b2][:], reads=[SfT[b2]])
                osqw = sb("osqw", [128, 512], F32, s2)
                orrw = sb("orrw", [128, 512], F32, s2)
                tmpT = [T(), T()]
                k.op("act", lambda e: e.activation(out=osqw[:], in_=ps[5][:], func=AF.Square), reads=[pst[5]], writes=[tmpT[0]])
                k.op("pe", lambda e: e.matmul(ps[7][:], lhsT=ones_f[:], rhs=osqw[:], start=True, stop=True),
                     reads=[tmpT[0]], writes=[pst[7]])
                k.op("act", lambda e: e.activation(out=orrw[:], in_=ps[7][:], func=AF.Sqrt, scale=1.0 / 128, bias=pv_eps[:, :]),
                     reads=[pst[7]], writes=[tmpT[1]])
                k.op("dve", lambda e: e.reciprocal(out=orrw[:], in_=orrw[:]), writes=[tmpT[1]])
                k.op("dve", lambda e: e.tensor_tensor(out=orrw[:], in0=ps[5][:], in1=orrw[:], op=ALU.mult),
                     reads=[pst[5]], writes=[tmpT[1]])
                k.op("dve", lambda e: e.scalar_tensor_tensor(out=cat[:, 8:16, 1024:1088],
                                                             in0=orrw[:].rearrange("p (h t) -> p h t", t=64), scalar=gn,
                                                             in1=sggS[:], op0=ALU.mult, op1=ALU.mult),
                     reads=[tmpT[1], sggST], writes=catTs[8:16])
                k.phase_end()
        stM.close()
        stC = ExitStack()
        acc = sb("acc", [128, 9, D], F32, stC)
        accT = [T() for _ in range(9)]
        comb = sb("comb", [128, 9, 32], F32, stC)
        combT = T()
        wk = sb("wk", [128, 9, 2], F32, stC)
        sloti = sb("sloti", [128, 9, 2], I32, stC)
        flagi = sb("flagi", [1, 2], I32, stC)
        dispT = T()
        h2T, h2Tt = cat, catT
        with ExitStack() as s3:
            gfr = sb("gfr", [128, D], F32, s3)
            gfrT = T()
            k.dma("sp", gfr[:], gffn_rep[:, :], writes=[gfrT])
            hbs = [sb("hbs%d" % i, [128, D], BF16, s3) for i in range(9)]
            hbsT = [T() for _ in range(9)]
            k.op("pool", lambda e: e.memset(hbs[8][64:128, :], 0.0), writes=[hbsT[8]])
            xres = [sb("xres%d" % i, [128, 512], F32, s3) for i in range(2)]
            xresT = [T() for _ in range(2)]
            wr = sb("wr", [128, NK, 36], BF16, s3)
            wrT = T()
            brt = sb("brt", [128, 36], F32, s3)
            k.dma("pool", wr[:], w_r_v, writes=[wrT])
            k.dma("sp", brt[:], b_r[:, :], writes=[wrT])
            for cb in range(4):
                wsl, wT = load_w(w_out_v, [(cb * 512, 512)])
                for tt, (r0, nr) in enumerate(TT):
                    b2 = tt % 2
                    pb = tt % 4
                    k.dma("sp", xres[b2][0:nr, :], x_main[r0:r0 + nr, cb * 512:(cb + 1) * 512], writes=[xresT[b2]])

                    def mmo(e):
                        ins = None
                        for kc in range(NK):
                            ins = e.matmul(ps[pb][0:nr, :], lhsT=cat[:, kc, r0:r0 + nr], rhs=wsl[:, kc, :],
                                           start=(kc == 0), stop=(kc == NK - 1))
                        return ins
                    k.op("pe", mmo, reads=catTs + [catT, wT], writes=[pst[pb]])
                    k.op("dve", lambda e: e.tensor_tensor(out=acc[0:nr, tt, cb * 512:(cb + 1) * 512], in0=ps[pb][0:nr, :],
                                                          in1=xres[b2][0:nr, :], op=ALU.add),
                         reads=[pst[pb], xresT[b2]], writes=[accT[tt]])
            Uu = sb("Uu", [128, 128], BF16, s3)
            ones_bf = sb("ones_bf", [128, 128], BF16, s3)
            base32 = sb("base32", [128, 32], F32, s3)
            ohA = sb("ohA", [128, 9, 32], F32, s3)
            ohB = sb("ohB", [128, 9, 32], F32, s3)
            sel = sb("sel", [128, 9, 32], BF16, s3)
            slotf = sb("slotf", [128, 9, 2], F32, s3)
            dcT = T()
            k.op("pool", lambda e: e.memset(Uu[:], 1.0), writes=[dcT])
            k.op("pool", lambda e: e.affine_select(out=Uu[:], in_=Uu[:], pattern=[[1, 128]], compare_op=ALU.is_gt, fill=0.0,
                                                   base=0, channel_multiplier=-1), writes=[dcT])
            k.op("pool", lambda e: e.memset(ones_bf[:], 1.0), writes=[dcT])
            k.op("pool", lambda e: e.iota(base32[:], pattern=[[CAP, 32]], base=0, channel_multiplier=0,
                                          allow_small_or_imprecise_dtypes=True), writes=[dcT])
            k.op("pool", lambda e: e.memset(sel[:], 0.0), writes=[dcT])
            k.op("pool", lambda e: e.memset(ohA[:], 0.0), writes=[dcT])
            k.op("pool", lambda e: e.memset(ohB[:], 0.0), writes=[dcT])
            k.op("pool", lambda e: e.memset(slotf[:], BIGSLOT), writes=[dcT])
            k.op("pool", lambda e: e.memset(wk[:], 0.0), writes=[dcT])
            junk = sb("junkC", [128, D], BF16, s3)
            junkT = T()
            ss = sb("ssC", [128, 4], F32, s3)
            ssT = T()
            for tt, (r0, nr) in enumerate(TT):
                P = slice(0, nr)
                k.op("act", lambda e: e.activation(out=junk[P, :], in_=acc[P, tt, :], func=AF.Square, accum_out=ss[P, 0:1]),
                     reads=[accT[tt]], writes=[junkT, ssT])
                k.op("act", lambda e: e.activation(out=ss[P, 1:2], in_=ss[P, 0:1], func=AF.Sqrt, scale=1.0 / D, bias=pv_eps[P, :]),
                     writes=[ssT])
                k.op("dve", lambda e: e.reciprocal(out=ss[P, 2:3], in_=ss[P, 1:2]), writes=[ssT])
                k.op("dve", lambda e: e.scalar_tensor_tensor(out=hbs[tt][P, :], in0=acc[P, tt, :], scalar=ss[P, 2:3], in1=gfr[P, :],
                                                             op0=ALU.mult, op1=ALU.mult),
                     reads=[accT[tt], ssT, gfrT], writes=[hbsT[tt]])
                for half in range(2):
                    pb = half
                    pbf = ps[pb][:].bitcast(BF16)

                    def tr(e):
                        ins = None
                        for j in range(8):
                            kc = half * 8 + j
                            ins = e.transpose(out=pbf[:, j * 128:j * 128 + nr], in_=hbs[tt][P, kc::NK],
                                              identity=ident_bf[P, P])
                        return ins
                    k.op("pe", tr, reads=[hbsT[tt]], writes=[pst[pb]])
                    k.op("act", lambda e: e.copy(out=h2T[:, half * 8:half * 8 + 8, r0:r0 + nr],
                                                 in_=pbf.rearrange("p (j t) -> p j t", t=128)[:, :, 0:nr]),
                         reads=[pst[pb]], writes=[h2Tt])

                def mmr(e):
                    ins = None
                    for kc in range(NK):
                        ins = e.matmul(ps[4][P, tt * 36:(tt + 1) * 36], lhsT=h2T[:, kc, r0:r0 + nr], rhs=wr[:, kc, :],
                                       start=(kc == 0), stop=(kc == NK - 1))
                    return ins
                k.op("pe", mmr, reads=[h2Tt, wrT], writes=[pst[4]])
            lgA = sb("lgA", [128, 9, 36], F32, s3)
            r9 = sb("r9", [128, 12, 9], F32, s3)
            w4 = sb("w4", [128, 2, 9, 4], F32, s3)
            w8 = sb("w8", [128, 5, 9, 8], F32, s3)
            rtT = T()

            def R(fn, e="dve", reads=()):
                k.op(e, fn, reads=list(reads), writes=[rtT])

            def bc(ap2, w):
                return ap2.unsqueeze(2).to_broadcast([128, 9, w])
            gmax, gsum, ptop, m1, m2, dd, ed, den = (r9[:, i, :] for i in range(8))
            ohg, ge = w4[:, 0], w4[:, 1]
            sel8, oh1, msk, oh2, t8 = (w8[:, i] for i in range(5))
            R(lambda e: e.tensor_tensor(out=lgA[:], in0=ps[4][:, 0:324].rearrange("p (a b) -> p a b", b=36),
                                        in1=brt[:].unsqueeze(1).to_broadcast([128, 9, 36]), op=ALU.add), reads=[pst[4], wrT])
            R(lambda e: e.tensor_reduce(out=gmax, in_=lgA[:, :, 0:4], axis=AX.X, op=ALU.max))
            R(lambda e: e.tensor_tensor(out=ohg, in0=lgA[:, :, 0:4], in1=bc(gmax, 4), op=ALU.is_equal))
            R(lambda e: e.tensor_tensor(out=ge, in0=lgA[:, :, 0:4], in1=bc(gmax, 4), op=ALU.subtract))
            R(lambda e: e.activation(out=ge, in_=ge, func=AF.Exp), "act")
            R(lambda e: e.tensor_reduce(out=gsum, in_=ge, axis=AX.X, op=ALU.add))
            R(lambda e: e.reciprocal(out=ptop, in_=gsum))
            R(lambda e: e.tensor_tensor(out=sel8, in0=lgA[:, :, 4:12], in1=bc(w4[:, 0, :, 0], 8), op=ALU.mult))
            for g in range(1, 4):
                R(lambda e: e.tensor_tensor(out=t8, in0=lgA[:, :, 4 + 8 * g:12 + 8 * g], in1=bc(w4[:, 0, :, g], 8), op=ALU.mult))
                R(lambda e: e.tensor_tensor(out=sel8, in0=sel8, in1=t8, op=ALU.add))
            R(lambda e: e.tensor_reduce(out=m1, in_=sel8, axis=AX.X, op=ALU.max))
            R(lambda e: e.tensor_tensor(out=oh1, in0=sel8, in1=bc(m1, 8), op=ALU.is_equal))
            R(lambda e: e.scalar_tensor_tensor(out=msk, in0=oh1, scalar=-1e30, in1=sel8, op0=ALU.mult, op1=ALU.add))
            R(lambda e: e.tensor_reduce(out=m2, in_=msk, axis=AX.X, op=ALU.max))
            R(lambda e: e.tensor_tensor(out=oh2, in0=msk, in1=bc(m2, 8), op=ALU.is_equal))
            R(lambda e: e.tensor_tensor(out=dd, in0=m2, in1=m1, op=ALU.subtract))
            R(lambda e: e.activation(out=ed, in_=dd, func=AF.Exp), "act")
            R(lambda e: e.tensor_scalar(out=den, in0=ed, scalar1=1.0, scalar2=None, op0=ALU.add))
            R(lambda e: e.reciprocal(out=den, in_=den))
            R(lambda e: e.tensor_tensor(out=wk[:, :, 0], in0=den, in1=ptop, op=ALU.mult), reads=[dcT])
            R(lambda e: e.tensor_tensor(out=wk[:, :, 1], in0=ed, in1=wk[:, :, 0], op=ALU.mult))
            for g in range(4):
                R(lambda e: e.tensor_tensor(out=ohA[:, :, 8 * g:8 * g + 8], in0=oh1, in1=bc(w4[:, 0, :, g], 8), op=ALU.mult))
                R(lambda e: e.tensor_tensor(out=ohB[:, :, 8 * g:8 * g + 8], in0=oh2, in1=bc(w4[:, 0, :, g], 8), op=ALU.mult))
            R(lambda e: e.memset(ohA[64:128, 8, :], 0.0))
            R(lambda e: e.memset(ohB[64:128, 8, :], 0.0))
            R(lambda e: e.memset(wk[64:128, 8, :], 0.0))
            R(lambda e: e.tensor_tensor(out=sel[:], in0=ohA[:], in1=ohB[:], op=ALU.add))
            k.op("dve", lambda e: e.tensor_tensor(out=comb[:], in0=ohA[:], in1=bc(wk[:, :, 0], 32), op=ALU.mult),
                 reads=[rtT], writes=[combT])
            k.op("dve", lambda e: e.tensor_tensor(out=lgA[:, :, 0:32], in0=ohB[:], in1=bc(wk[:, :, 1], 32), op=ALU.mult),
                 reads=[rtT], writes=[rtT])
            k.op("dve", lambda e: e.tensor_tensor(out=comb[:], in0=comb[:], in1=lgA[:, :, 0:32], op=ALU.add),
                 reads=[rtT], writes=[combT])
            tmp32 = sb("tmp32", [128, 9, 32], F32, s3)
            prod = sb("prod", [128, 9, 32], F32, s3)
            flg = sb("flg", [128, 2], F32, s3)

            def mmp(e):
                ins = None
                for tt in range(9):
                    for t2 in range(tt):
                        ins = e.matmul(ps[6][:, tt * 32:(tt + 1) * 32], lhsT=ones_bf[:], rhs=sel[:, t2, :], start=(t2 == 0), stop=False)
                    ins = e.matmul(ps[6][:, tt * 32:(tt + 1) * 32], lhsT=Uu[:], rhs=sel[:, tt, :], start=(tt == 0), stop=True)
                return ins
            k.op("pe", mmp, reads=[rtT, dcT], writes=[pst[6]])
            pos = ps[6][:, 0:288].rearrange("p (a b) -> p a b", b=32)
            R(lambda e: e.tensor_scalar(out=prod[:], in0=pos, scalar1=float(CAP), scalar2=BIGSLOT, op0=ALU.is_ge, op1=ALU.mult),
              reads=[pst[6]])
            R(lambda e: e.tensor_tensor(out=tmp32[:], in0=pos, in1=base32[:].unsqueeze(1).to_broadcast([128, 9, 32]), op=ALU.add),
              reads=[pst[6], dcT])
            R(lambda e: e.tensor_tensor(out=tmp32[:], in0=tmp32[:], in1=prod[:], op=ALU.add))
            for kk, oh in enumerate((ohA, ohB)):
                R(lambda e: e.tensor_tensor(out=prod[:], in0=oh[:], in1=tmp32[:], op=ALU.mult))
                R(lambda e: e.tensor_reduce(out=slotf[:, :, kk], in_=prod[:], axis=AX.X, op=ALU.add))
            R(lambda e: e.memset(slotf[64:128, 8, :], BIGSLOT))
            R(lambda e: e.tensor_copy(out=sloti[:], in_=slotf[:]))
            R(lambda e: e.tensor_scalar(out=slotf[:], in0=slotf[:], scalar1=BIGSLOT, scalar2=None, op0=ALU.is_ge))
            R(lambda e: e.memset(slotf[64:128, 8, :], 0.0))
            R(lambda e: e.tensor_reduce(out=flg[:, 0:1], in_=slotf[:].rearrange("p a b -> p (a b)"), axis=AX.X, op=ALU.add))
            k.op("pe", lambda e: e.matmul(ps[5][0:1, 0:1], lhsT=flg[:, 0:1], rhs=ones_f[:, 0:1], start=True, stop=True),
                 reads=[rtT], writes=[pst[5]])
            k.op("dve", lambda e: e.tensor_copy(out=flagi[0:1, 0:1], in_=ps[5][0:1, 0:1]), reads=[pst[5]], writes=[dispT])
            for tt in range(9):
                for kk in range(2):
                    k.dma("pool", None, None, reads=[hbsT[tt], rtT], writes=[XsT] + XsZ,
                          fn=lambda e: e.indirect_dma_start(out=Xs[:, :], out_offset=bass.IndirectOffsetOnAxis(ap=sloti[:, tt, kk:kk + 1], axis=0),
                                                            in_=hbs[tt][:], in_offset=None, bounds_check=NE * CAP - 1, oob_is_err=False))
            precast(1000)
            k.phase_end()

        stW = ExitStack()
        slot3 = sb("wslot2", [128, NK, 512], BF16, stW)
        gu = [wslots[0], wslots[1], slot3]
        guT = [wslotT[0], wslotT[1], T()]
        wd = [sb("wd%d" % i, [128, 4, D], BF16, stW) for i in range(2)]
        wdT = [T() for _ in range(2)]
        gctr = [0]

        def load_expert(ex, parts="gud"):
            sgi = (2 * ex) % 3
            sui = (2 * ex + 1) % 3
            di = ex % 2
            if ex < NPRE:
                q_ = "sp"
                sg_, su_, sd_ = wg_bf[ex], wu_bf[ex], wd_bf[ex]
            else:
                q_ = "pool"
                sg_ = w_gate[ex].rearrange("(p kc) f -> p kc f", kc=NK)
                su_ = w_up[ex].rearrange("(p kc) f -> p kc f", kc=NK)
                sd_ = w_down[ex].rearrange("(fc p) d -> p fc d", p=128)
            kw_ = {} if ex < NPRE else {"max_dma_last_dim": 4096}
            if "g" in parts:
                k.dma(q_, gu[sgi][:], sg_, reads=[pcT[ex][0]], writes=[guT[sgi]], **kw_)
            if "u" in parts:
                k.dma(q_, gu[sui][:], su_, reads=[pcT[ex][1]], writes=[guT[sui]], **kw_)
            if "d" in parts:
                k.dma(q_, wd[di][:], sd_, reads=[pcT[ex][2]], writes=[wdT[di]])
            return sgi, sui, di

        with ExitStack() as s4:
            Xe2 = [sb("Xe%d" % i, [128, D], BF16, s4) for i in range(2)]
            XeTk2 = [T(), T()]
            XeT_ = sb("XeT", [128, NK, CAP], BF16, s4)
            hidS = sb("hidS", [128, 4, CAP], BF16, s4)
            hidTM = sb("hidTM", [128, 512], BF16, s4)
            hidTMT = T()
            Yo = sb("Yo", [128, D], BF16, s4)
            XeTT, hidST, YoT = T(), T(), T()
            sgW, sgWT = Yo[:, 0:1024].bitcast(F32), YoT
            precast(1000)
            load_expert(0)
            k.dma("sp", Xe2[0][:], Xs[0:CAP, :], reads=[XsT], writes=[XeTk2[0]])

            def emit_T(ex):
                Xe, XeTk = Xe2[ex % 2], XeTk2[ex % 2]
                pbf = ps[0][:].bitcast(BF16)
                for half in range(2):
                    def tr(e):
                        ins = None
                        for j in range(8):
                            kc = half * 8 + j
                            ins = e.transpose(out=pbf[:, j * 128:(j + 1) * 128], in_=Xe[:, kc::NK], identity=ident_bf[:])
                        return ins
                    k.op("pe", tr, reads=[XeTk], writes=[pst[0]])
                    k.op("act", lambda e: e.copy(out=XeT_[:, half * 8:half * 8 + 8, :], in_=pbf.rearrange("p (j t) -> p j t", t=128)),
                         reads=[pst[0]], writes=[XeTT])

            emit_T(0)
            for ex in range(NE):
                sgi, sui, di = load_expert(ex, "")
                if ex + 1 < NE:
                    k.dma("sp", Xe2[(ex + 1) % 2][:], Xs[(ex + 1) * CAP:(ex + 2) * CAP, :], reads=[XsT], writes=[XeTk2[(ex + 1) % 2]])
                    load_expert(ex + 1, "gd")
                bq = 2 + 2 * (ex % 2)

                def mmgu(e):
                    ins = None
                    for kc in range(NK):
                        e.matmul(ps[bq][:, :], lhsT=XeT_[:, kc, :], rhs=gu[sgi][:, kc, :], start=(kc == 0), stop=(kc == NK - 1))
                        ins = e.matmul(ps[bq + 1][:, :], lhsT=XeT_[:, kc, :], rhs=gu[sui][:, kc, :], start=(kc == 0),
                                       stop=(kc == NK - 1))
                    return ins
                k.op("pe", mmgu, reads=[XeTT, guT[sgi], guT[sui]], writes=[pst[bq], pst[bq + 1]])
                k.op("act", lambda e: e.activation(out=sgW, in_=ps[bq][:, :], func=AF.Silu), reads=[pst[bq]], writes=[sgWT])
                k.op("dve", lambda e: e.tensor_tensor(out=hidTM[:], in0=sgW, in1=ps[bq + 1][:, :], op=ALU.mult),
                     reads=[sgWT, pst[bq + 1]], writes=[hidTMT])
                if ex + 1 < NE:
                    load_expert(ex + 1, "u")
                    emit_T(ex + 1)
                pbfh = ps[1][:].bitcast(BF16)

                def trh(e):
                    ins = None
                    for fc in range(4):
                        ins = e.transpose(out=pbfh[:, fc * 128:(fc + 1) * 128], in_=hidTM[:, fc * 128:(fc + 1) * 128], identity=ident_bf[:])
                    return ins
                k.op("pe", trh, reads=[hidTMT], writes=[pst[1]])
                k.op("act", lambda e: e.copy(out=hidS[:], in_=pbfh[:, 0:512].rearrange("p (j t) -> p j t", t=128)),
                     reads=[pst[1]], writes=[hidST])
                for db in range(4):
                    py = 6 + db % 2

                    def mmd(e):
                        ins = None
                        for fc in range(4):
                            ins = e.matmul(ps[py][:, :], lhsT=hidS[:, fc, :], rhs=wd[di][:, fc, db * 512:(db + 1) * 512],
                                           start=(fc == 0), stop=(fc == 3))
                        return ins
                    k.op("pe", mmd, reads=[hidST, wdT[di]], writes=[pst[py]])
                    k.op("act", lambda e: e.copy(out=Yo[:, db * 512:(db + 1) * 512], in_=ps[py][:, :]), reads=[pst[py]], writes=[YoT])
                k.dma("act", Ys[ex * CAP:(ex + 1) * CAP, :], Yo[:], reads=[YoT], writes=[YsT])
            k.soft_barrier()

        def final_store(views):
            nfb, ot0, ot1, junk = views
            nfbT = T()
            ot = [ot0, ot1]
            otT = [T(), T()]
            junkT = T()
            ssT = T()
            k.dma("sp", nfb, nfin[:, :], writes=[nfbT])
            for tt, (r0, nr) in enumerate(TT):
                b2 = tt % 2
                k.op("act", lambda e: e.activation(out=junk[0:nr, :], in_=acc[0:nr, tt, :], func=AF.Square, accum_out=ssE[0:nr, 0:1]),
                     reads=[accT[tt]], writes=[junkT, ssT])
                k.op("act", lambda e: e.activation(out=ssE[0:nr, 1:2], in_=ssE[0:nr, 0:1], func=AF.Sqrt, scale=1.0 / D,
                                                   bias=pv_eps[0:nr, :]), writes=[ssT])
                k.op("dve", lambda e: e.reciprocal(out=ssE[0:nr, 2:3], in_=ssE[0:nr, 1:2]), writes=[ssT])
                k.op("dve", lambda e: e.scalar_tensor_tensor(out=ot[b2][0:nr, :], in0=acc[0:nr, tt, :], scalar=ssE[0:nr, 2:3],
                                                             in1=nfb[0:nr, :], op0=ALU.mult, op1=ALU.mult),
                     reads=[accT[tt], ssT, nfbT], writes=[otT[b2]])
                k.dma("sp", y_main[r0:r0 + nr, :], ot[b2][0:nr, :], reads=[otT[b2]])
            k.drain_dmas("sp")

        ssE = sb("ssE", [128, 4], F32, stW)
        wdf = [w_[:].rearrange("p a b -> p (a b)").bitcast(F32) for w_ in wd]
        s3f = slot3[:].rearrange("p a b -> p (a b)")
        regs = {e: stW.enter_context(k.eng[e].register("rf_" + e)) for e in ("pe", "act", "dve", "pool", "sp")}
        for e in regs:
            k._wait(e, [dispT.w])
            k.eng[e].reg_load(regs[e], flagi[0:1, 0:1])
        snap = k.snapshot()
        if FORCE_FALLBACK:
            cmpv = 7
        else:
            cmpv = 0
        k.start_defer()
        if True:
            wd0 = wd[0][:].rearrange("p a b -> p (a b)")
            G = [wd0[:, 0:D], wd0[:, D:2 * D]]
            GT = [T(), T()]
            for tt, (r0, nr) in enumerate(TT):
                for kk in range(2):
                    k.dma("pool", None, None, reads=[YsT], writes=[GT[kk]],
                          fn=lambda e: e.indirect_dma_start(out=G[kk], out_offset=None, in_=Ys[:, :],
                                                            in_offset=bass.IndirectOffsetOnAxis(ap=sloti[:, tt, kk:kk + 1], axis=0),
                                                            bounds_check=NE * CAP - 1, oob_is_err=False))
                    k.op("dve", lambda e: e.scalar_tensor_tensor(out=acc[0:nr, tt, :], in0=G[kk][0:nr, :], scalar=wk[0:nr, tt, kk:kk + 1],
                                                                 in1=acc[0:nr, tt, :], op0=ALU.mult, op1=ALU.add),
                         reads=[GT[kk]], writes=[accT[tt]])
            final_store((wdf[1][:, 0:D], wdf[1][:, D:2 * D], s3f[:, 0:2 * D].bitcast(F32), s3f[:, 2 * D:3 * D]))
        listA = k.end_defer()
        k.restore(snap)
        k.start_defer()
        if True:
            with ExitStack() as s4:
                hid = sb("hid", [128, 4, TV], BF16, s4)
                hidT = T()
                sgt = [sb("sgt%d" % i, [128, 512], F32, s4) for i in range(2)]
                sgtT = [T() for _ in range(2)]
                mctr = 0
                yctr = 0
                for ex in range(NE):
                    sgi, sui, di = load_expert(ex)
                    for (t0, n) in NT:
                        for fc in range(4):
                            pa = mctr % 2
                            pu = 2 + mctr % 2
                            mctr += 1

                            def mmg_(e):
                                ins = None
                                for kc in range(NK):
                                    ins = e.matmul(ps[pa][:, 0:n], lhsT=gu[sgi][:, kc, fc * 128:(fc + 1) * 128],
                                                   rhs=h2T[:, kc, t0:t0 + n], start=(kc == 0), stop=(kc == NK - 1))
                                return ins

                            def mmu_(e):
                                ins = None
                                for kc in range(NK):
                                    ins = e.matmul(ps[pu][:, 0:n], lhsT=gu[sui][:, kc, fc * 128:(fc + 1) * 128],
                                                   rhs=h2T[:, kc, t0:t0 + n], start=(kc == 0), stop=(kc == NK - 1))
                                return ins
                            k.op("pe", mmg_, reads=[h2Tt, guT[sgi]], writes=[pst[pa]])
                            k.op("pe", mmu_, reads=[h2Tt, guT[sui]], writes=[pst[pu]])
                            s_ = pa
                            k.op("act", lambda e: e.activation(out=sgt[s_][:, 0:n], in_=ps[pa][:, 0:n], func=AF.Silu),
                                 reads=[pst[pa]], writes=[sgtT[s_]])
                            k.op("dve", lambda e: e.tensor_tensor(out=hid[:, fc, t0:t0 + n], in0=sgt[s_][:, 0:n], in1=ps[pu][:, 0:n],
                                                                  op=ALU.mult), reads=[sgtT[s_], pst[pu]], writes=[hidT])
                    for tt, (r0, nr) in enumerate(TT):
                        for db in range(4):
                            py = 4 + yctr % 4
                            yctr += 1

                            def mmd(e):
                                ins = None
                                for fc in range(4):
                                    ins = e.matmul(ps[py][0:nr, :], lhsT=hid[:, fc, r0:r0 + nr], rhs=wd[di][:, fc, db * 512:(db + 1) * 512],
                                                   start=(fc == 0), stop=(fc == 3))
                                return ins
                            k.op("pe", mmd, reads=[hidT, wdT[di]], writes=[pst[py]])
                            k.op("dve", lambda e: e.scalar_tensor_tensor(out=acc[0:nr, tt, db * 512:(db + 1) * 512], in0=ps[py][0:nr, :],
                                                                         scalar=comb[0:nr, tt, ex:ex + 1],
                                                                         in1=acc[0:nr, tt, db * 512:(db + 1) * 512],
                                                                         op0=ALU.mult, op1=ALU.add),
                                 reads=[pst[py], combT], writes=[accT[tt]])
                k.soft_barrier()
            final_store((wdf[1][:, 0:D], wdf[1][:, D:2 * D], wdf[0][:, 0:D], s3f[:, 2 * D:3 * D]))
        listB = k.end_defer()
        for e in regs:
            if not listA[e] and not listB[e]:
                continue
            with k.eng[e].If_eq(regs[e], cmpv):
                k.replay(e, listA[e])
            with k.eng[e].Else():
                k.replay(e, listB[e])
        es.pop_all()
    return nc


def _pvec(inp):
    def fm(v, n):
        return np.ascontiguousarray(np.asarray(v, np.float32).reshape(n, 128).T)
    wdw = np.asarray(inp["w_dw"][0], np.float32)
    wdw_fm = np.ascontiguousarray(wdw.reshape(CONVW, 8, 128).transpose(2, 1, 0)).reshape(128, 8 * CONVW)
    cols = [fm(inp["norm_mix"][0], 16), fm(inp["norm_ffn"][0], 16), fm(inp["b_dw"][0], 8), fm(inp["ln_conv_g"][0], 8),
            fm(inp["ln_conv_b"][0], 8), fm(inp["lb_logits"][0], 8), fm(inp["lb_logits"][1], 8),
            np.asarray(inp["hgrn_norm_g"][0], np.float32).reshape(128, 1), wdw_fm]
    return np.ascontiguousarray(np.concatenate(cols, axis=1))


_NC_CACHE = {}
STAGE = 9


def kernel(**inp):
    inp = {k_: np.asarray(v) for k_, v in inp.items()}
    if "nc" not in _NC_CACHE:
        _NC_CACHE["nc"] = build_program(STAGE)
    nc = _NC_CACHE["nc"]
    xp = inp["x_prompt"].astype(np.float32, copy=False)
    xs = inp["x_sample"].astype(np.float32, copy=False).reshape(128 * 4, D)
    pv = _pvec(inp)
    w_r = np.ascontiguousarray(np.concatenate([inp["w_router_group"][0], inp["w_router_expert"][0]], axis=1))
    b_r = np.ascontiguousarray(np.broadcast_to(np.concatenate([inp["b_router_group"][0], inp["b_router_expert"][0]])[None, :], (128, 36)))
    shared = {
        "pvec": pv, "nfin": np.ascontiguousarray(np.broadcast_to(inp["norm_final"][None, :], (128, D))),
        "gffn_rep": np.ascontiguousarray(np.broadcast_to(inp["norm_ffn"][0][None, :], (128, D))),
        "zeros_d": np.zeros((128, D), ml_dtypes.bfloat16),
        "w_in": np.ascontiguousarray(inp["w_in"][0]), "w_out": np.ascontiguousarray(inp["w_out"][0]),
        "w_r": w_r, "b_r": b_r,
    }
    if True:
        shared["w_gate"] = np.ascontiguousarray(inp["w_exp_gate"][0])
        shared["w_up"] = np.ascontiguousarray(inp["w_exp_up"][0])
        shared["w_down"] = np.ascontiguousarray(inp["w_exp_down"][0])
    in_maps = []
    for c in range(NCORES):
        b, hf = c // 2, c % 2
        cur = xp[b, hf * 1024:(hf + 1) * 1024]
        if hf == 1:
            prev = xp[b, 0:1024]
        else:
            prev = np.zeros((1024, D), np.float32)
        xm = np.concatenate([cur, xs[c * 64:(c + 1) * 64], prev[1024 - THALO:]], axis=0)
        m = dict(shared)
        m["x_main"] = np.ascontiguousarray(xm)
        m["x_prev"] = np.ascontiguousarray(prev)
        m["sconv"] = np.ascontiguousarray(inp["state_conv"][0, c * 16:(c + 1) * 16])
        m["shgrn"] = np.ascontiguousarray(inp["state_hgrn"][0, c * 16:(c + 1) * 16])
        in_maps.append(m)
    res = run_bass_kernel_spmd(nc, in_maps, core_ids=list(range(NCORES)))
    R = res.results
    y_prompt = np.zeros((4, 2048, D), np.float32)
    y_sample = np.zeros((128, 4, D), np.float32)
    ncp = np.zeros((1, 4, 30, 1024), np.float32)
    nhp = np.zeros((1, 4, 8, 128, 128), np.float32)
    ncs = np.zeros((1, 128, 30, 1024), np.float32)
    nhs = np.zeros((1, 128, 8, 128, 128), np.float32)
    for c in range(NCORES):
        b, hf = c // 2, c % 2
        ym = R[c]["y_main"]
        y_prompt[b, hf * 1024:(hf + 1) * 1024] = ym[0:1024]
        y_sample[c * 16:(c + 1) * 16] = ym[1024:1088].reshape(16, 4, D)
        if hf == 1:
            ncp[0, b] = R[c]["conv_p"]
            nhp[0, b] = R[c]["hgrn_p"]
        ncs[0, c * 16:(c + 1) * 16] = R[c]["conv_s"]
        nhs[0, c * 16:(c + 1) * 16] = R[c]["hgrn_s"]
    return (y_prompt, y_sample, ncp, nhp, ncs, nhs)
```

```python
import numpy as np
import ml_dtypes
from contextlib import ExitStack
import concourse.bass as bass
import concourse.mybir as mybir
from concourse.bass_utils import run_bass_kernel_spmd

F32 = mybir.dt.float32
BF16 = mybir.dt.bfloat16
AF = mybir.ActivationFunctionType
ALU = mybir.AluOpType
AX = mybir.AxisListType

NCORES = 8
D = 2048
NK = 16
TCUR = 1024
TS = 64
THALO = 32
TV = TCUR + TS
TM = TV + THALO
TPREV = 1024
CONVW = 31
NE = 32
DE = 512
EPS = 1e-6
CAP = 128
NPRE = 20
BIGSLOT = 1.0e6
FORCE_FALLBACK = False
I32 = mybir.dt.int32
NPV = 16 + 16 + 8 + 8 + 8 + 8 + 8 + 1 + 8 * CONVW
PV_GMIX, PV_GFFN, PV_BDW, PV_LNG, PV_LNB, PV_L0, PV_L1, PV_GN, PV_WDW = 0, 16, 32, 40, 48, 56, 64, 72, 73


class Tk:
    __slots__ = ("sem", "name", "val", "eng")

    def __init__(self, sem, name, val, eng):
        self.sem, self.name, self.val, self.eng = sem, name, val, eng


class T:
    __slots__ = ("w", "r", "name")

    REG = []

    def __init__(self, name=""):
        self.w = None
        self.r = []
        self.name = name
        T.REG.append(self)


class _Dummy:
    def then_inc(self, *a, **kw):
        return self


class _Rec:
    def __init__(self):
        self.calls = []

    def __getattr__(self, name):
        def f(*a, **kw):
            self.calls.append((name, a, kw))
            return _Dummy()
        return f


class K:
    NDS = 12
    defer = None

    def __init__(self, nc, es):
        self.nc = nc
        self.eng = {"pe": nc.tensor, "act": nc.scalar, "dve": nc.vector, "pool": nc.gpsimd, "sp": nc.sync}
        self.sem = {}
        self.cnt = {}
        self.seen = {e: {} for e in self.eng}
        for e in ("pe", "act", "dve", "pool"):
            self.sem[e] = es.enter_context(nc.semaphore("s_" + e))
            self.cnt[e] = 0
        self.dsem = {}
        self.dcnt = {}
        self.dnext = {}
        for q in ("sp", "pool", "act", "poolbg", "spbg"):
            n = self.NDS if q not in ("act",) else 4
            self.dsem[q] = [es.enter_context(nc.semaphore("d_%s%d" % (q, i))) for i in range(n)]
            self.dcnt[q] = [0] * n
            self.dnext[q] = 0

    def _wait(self, e, tks):
        seen = self.seen[e]
        for tk in tks:
            if tk is None:
                continue
            if e == "pe" and tk.eng == "pe":
                continue
            if seen.get(tk.name, 0) >= tk.val:
                continue
            if self.defer is not None:
                self.defer[e].append(("w", tk.sem, tk.val))
            else:
                self.eng[e].wait_ge(tk.sem, tk.val)
            seen[tk.name] = tk.val

    def _deps(self, reads, writes):
        deps = []
        for t in reads:
            deps.append(t.w)
        for t in writes:
            deps.append(t.w)
            deps.extend(t.r)
        return deps

    def snapshot(self):
        return ({e: c for e, c in self.cnt.items()}, {q: list(v) for q, v in self.dcnt.items()}, dict(self.dnext),
                {e: dict(v) for e, v in self.seen.items()}, [(t, t.w, list(t.r)) for t in T.REG])

    def restore(self, snap):
        cnt, dcnt, dnext, seen, tiles = snap
        self.cnt = dict(cnt)
        self.dcnt = {q: list(v) for q, v in dcnt.items()}
        self.dnext = dict(dnext)
        self.seen = {e: dict(v) for e, v in seen.items()}
        for (t, w, r) in tiles:
            t.w = w
            t.r = list(r)

    def start_defer(self):
        self.defer = {e: [] for e in self.eng}

    def end_defer(self):
        d = self.defer
        self.defer = None
        return d

    def replay(self, e, items):
        eng = self.eng[e]
        if not items:
            eng.nop()
        for it in items:
            if it[0] == "w":
                eng.wait_ge(it[1], it[2])
            else:
                inst = None
                for (name, a, kw) in it[1]:
                    inst = getattr(eng, name)(*a, **kw)
                inst.then_inc(it[2], it[3])

    def soft_barrier(self):
        for e in self.eng:
            tks = [Tk(self.sem[x], x, self.cnt[x], x) for x in self.sem if self.cnt[x] > 0 and x != e]
            self._wait(e, tks)
            self.drain_dmas(e)

    def op(self, e, fn, reads=(), writes=()):
        self._wait(e, [self.gconst.w] if getattr(self, "gconst", None) is not None else [])
        self._wait(e, self._deps(reads, writes))
        if self.defer is not None:
            rec = _Rec()
            fn(rec)
            self.defer[e].append(("i", rec.calls, self.sem[e], 1))
        else:
            inst = fn(self.eng[e])
            inst.then_inc(self.sem[e], 1)
        self.cnt[e] += 1
        tk = Tk(self.sem[e], e, self.cnt[e], e)
        for t in reads:
            t.r.append(tk)
        for t in writes:
            t.w = tk
            t.r = []
        return tk

    def dma(self, q, out, in_, reads=(), writes=(), fn=None, bg=False, **kw):
        sq = q + "bg" if bg else q
        self._wait(q, [self.gconst.w] if getattr(self, "gconst", None) is not None else [])
        self._wait(q, self._deps(reads, writes))
        i = self.dnext[sq]
        self.dnext[sq] = (i + 1) % len(self.dsem[sq])
        sem = self.dsem[sq][i]
        name = "d_%s%d" % (sq, i)
        if self.dcnt[sq][i] > 0:
            self._wait(q, [Tk(sem, name, self.dcnt[sq][i], "dma")])
        if self.defer is not None:
            rec = _Rec()
            if fn is not None:
                fn(rec)
            else:
                rec.dma_start(out=out, in_=in_, **kw)
            self.defer[q].append(("i", rec.calls, sem, 16))
        elif fn is not None:
            fn(self.eng[q]).then_inc(sem, 16)
        else:
            self.eng[q].dma_start(out=out, in_=in_, **kw).then_inc(sem, 16)
        self.dcnt[sq][i] += 16
        tk = Tk(sem, name, self.dcnt[sq][i], "dma")
        for t in reads:
            t.r.append(tk)
        for t in writes:
            t.w = tk
            t.r = []
        return tk

    def drain_dmas(self, e="sp", bg=False):
        for q in self.dsem:
            if q.endswith("bg") and not bg:
                continue
            for i, sem in enumerate(self.dsem[q]):
                if self.dcnt[q][i] > 0:
                    self._wait(e, [Tk(sem, "d_%s%d" % (q, i), self.dcnt[q][i], "dma")])

    def phase_end(self):
        self.drain_dmas("sp")
        self.nc.all_engine_barrier()


def build_program(stage=9):
    nc = bass.Bass("TRN2", target_bir_lowering=False)

    def din(name, shape):
        return nc.dram_tensor(name, list(shape), F32, kind="ExternalInput").ap()

    def dout(name, shape):
        return nc.dram_tensor(name, list(shape), F32, kind="ExternalOutput").ap()

    x_main = din("x_main", [TM, D])
    x_prev = din("x_prev", [TPREV, D])
    sconv = din("sconv", [16, 30, 1024])
    shgrn = din("shgrn", [16, 8, 128, 128])
    pvec = din("pvec", [128, NPV])
    nfin = din("nfin", [128, D])
    gffn_rep = din("gffn_rep", [128, D])
    zeros_d = nc.dram_tensor("zeros_d", [128, D], BF16, kind="ExternalInput").ap()
    w_in = din("w_in", [D, 6144])
    w_out = din("w_out", [D, D])
    w_r = din("w_r", [D, 36])
    b_r = din("b_r", [128, 36])
    if True:
        w_gate = din("w_gate", [NE, D, DE])
        w_up = din("w_up", [NE, D, DE])
        w_down = din("w_down", [NE, DE, D])

    y_main = dout("y_main", [TV, D])
    conv_p = dout("conv_p", [30, 1024])
    hgrn_p = dout("hgrn_p", [8, 128, 128])
    conv_s = dout("conv_s", [16, 30, 1024])
    hgrn_s = dout("hgrn_s", [16, 8, 128, 128])

    w_in_v = w_in.rearrange("(kc p) c -> p kc c", p=128)
    w_out_v = w_out.rearrange("(kc p) c -> p kc c", p=128)
    w_r_v = w_r.rearrange("(p kc) c -> p kc c", kc=NK)

    es = ExitStack()
    with es:
        k = K(nc, es)

        def sb(name, shape, dt=F32, stack=None):
            return (stack or es).enter_context(nc.sbuf_tensor(name, list(shape), dt))

        ps = [es.enter_context(nc.psum_tensor("ps%d" % i, [128, 512], F32)) for i in range(8)]
        pst = [T("ps%d" % i) for i in range(8)]

        ident_bf = sb("ident_bf", [128, 128], BF16)
        ones_f = sb("ones_f", [128, 128], F32)
        pv = sb("pv", [128, NPV], F32)
        lb = sb("lb", [128, 8], F32)
        oml = sb("oml", [128, 8], F32)
        pv_eps = sb("pv_eps", [128, 1], F32)
        wslots = [sb("wslot%d" % i, [128, NK, 512], BF16) for i in range(2)]
        cat = sb("cat", [128, NK, TV], BF16)
        catT = T()
        catTs = [T() for _ in range(NK)]
        stM = ExitStack()
        ident_f = sb("ident_f", [128, 128], F32, stM)
        maskP = sb("maskP", [128, 128], F32, stM)
        maskS = sb("maskS", [64, 64], F32, stM)
        rmask = sb("rmask", [128, TV], F32, stM)
        gxm = sb("gxm", [128, NK, 128], F32, stM)
        t_const = T("const")
        k.gconst = t_const
        S = sb("S", [128, 8, 128], F32, stM)
        S_bf = sb("S_bf", [128, 8, 128], BF16, stM)
        tS = [T("S%d" % h) for h in range(8)]
        acc = None

        def c_(fn, e="pool"):
            k.op(e, fn, writes=[t_const])

        k.dma("sp", pv[:], pvec[:, :], writes=[t_const])
        c_(lambda e: e.memset(ident_bf[:], 1.0))
        c_(lambda e: e.affine_select(out=ident_bf[:], in_=ident_bf[:], pattern=[[-1, 128]], compare_op=ALU.is_equal,
                                     fill=0.0, base=0, channel_multiplier=1))
        c_(lambda e: e.memset(ident_f[:], 1.0))
        c_(lambda e: e.affine_select(out=ident_f[:], in_=ident_f[:], pattern=[[-1, 128]], compare_op=ALU.is_equal,
                                     fill=0.0, base=0, channel_multiplier=1))
        c_(lambda e: e.memset(ones_f[:], 1.0))
        c_(lambda e: e.memset(pv_eps[:], EPS))
        c_(lambda e: e.memset(maskP[:], 1.0))
        c_(lambda e: e.affine_select(out=maskP[:], in_=maskP[:], pattern=[[1, 128]], compare_op=ALU.is_ge,
                                     fill=0.0, base=0, channel_multiplier=-1))
        c_(lambda e: e.memset(maskP[0:64, 64:128], 0.0))
        c_(lambda e: e.memset(maskS[:], 1.0))
        c_(lambda e: e.affine_select(out=maskS[:], in_=maskS[:], pattern=[[4, 16], [1, 4]], compare_op=ALU.is_ge,
                                     fill=0.0, base=0, channel_multiplier=-1))
        c_(lambda e: e.affine_select(out=maskS[:], in_=maskS[:], pattern=[[-4, 16], [0, 4]], compare_op=ALU.is_ge,
                                     fill=0.0, base=0, channel_multiplier=1))
        c_(lambda e: e.memset(rmask[:], 1.0))
        c_(lambda e: e.memset(rmask[:, 0:TCUR].rearrange("p (c j) -> p c j", j=64)[:, :, 0:1], 0.0))
        c_(lambda e: e.memset(rmask[:, TCUR:TV].rearrange("p (c j) -> p c j", j=4)[:, :, 0:1], 0.0))
        c_(lambda e: e.memset(S[:], 0.0))
        c_(lambda e: e.memset(S_bf[:], 0.0))
        c_(lambda e: e.tensor_tensor(out=lb[:], in0=pv[:, PV_L0:PV_L0 + 8], in1=pv[:, PV_L1:PV_L1 + 8], op=ALU.subtract), "dve")
        c_(lambda e: e.activation(out=lb[:], in_=lb[:], func=AF.Sigmoid), "act")
        c_(lambda e: e.tensor_scalar(out=oml[:], in0=lb[:], scalar1=-1.0, scalar2=1.0, op0=ALU.mult, op1=ALU.add), "dve")
        for kc in range(NK):
            c_(lambda e: e.tensor_scalar(out=gxm[:, kc, :], in0=ones_f[:], scalar1=pv[:, PV_GMIX + kc:PV_GMIX + kc + 1],
                                         scalar2=None, op0=ALU.mult), "dve")
        Xs = nc.dram_tensor("Xs", [NE * CAP, D], BF16, kind="Internal").ap()
        Ys = nc.dram_tensor("Ys", [NE * CAP, D], BF16, kind="Internal").ap()
        XsT = T()
        YsT = T()
        XsZ = [T() for _ in range(NE * CAP // 128)]
        for ex in range(NE * CAP // 128):
            k.dma("sp", Xs[ex * 128:(ex + 1) * 128, :], zeros_d[:, :], writes=[XsZ[ex]], bg=True)
        k.phase_end()

        def load_norm_transpose(st, xsrc, nrows_list, hT, gx, tag, sbsrc=None, hTt=None):
            if sbsrc is None:
                xt = [sb("xt%s%d" % (tag, i), [128, D], F32, st) for i in range(2)]
                xtT = [T() for _ in range(2)]
            hb = [sb("hb%s%d" % (tag, i), [128, D], BF16, st) for i in range(2)]
            hbT = [T() for _ in range(2)]
            junk = sb("junk" + tag, [128, D], BF16, st)
            junkT = T()
            ss = sb("ss" + tag, [128, 4], F32, st)
            ssT = T()
            if hTt is None:
                hTt = T()
            row = 0
            for i, nr in enumerate(nrows_list):
                b = i % 2
                if sbsrc is None:
                    k.dma("sp", xt[b][0:nr, :], xsrc[row:row + nr, :], writes=[xtT[b]])
                    xin, xinT = xt[b][0:nr, :], xtT[b]
                else:
                    xin, xinT = sbsrc[0][0:nr, i, :], sbsrc[1][i]
                k.op("act", lambda e: e.activation(out=junk[0:nr, :], in_=xin, func=AF.Square,
                                                   accum_out=ss[0:nr, 0:1]), reads=[xinT], writes=[junkT, ssT])
                k.op("act", lambda e: e.activation(out=ss[0:nr, 1:2], in_=ss[0:nr, 0:1], func=AF.Sqrt, scale=1.0 / D,
                                                   bias=pv_eps[0:nr, :]), writes=[ssT])
                k.op("dve", lambda e: e.reciprocal(out=ss[0:nr, 2:3], in_=ss[0:nr, 1:2]), writes=[ssT])
                k.op("act", lambda e: e.activation(out=hb[b][0:nr, :], in_=xin, func=AF.Copy,
                                                   scale=ss[0:nr, 2:3]), reads=[xinT, ssT], writes=[hbT[b]])
                for half in range(2):
                    pb = half
                    pbf = ps[pb][:].bitcast(BF16)

                    def tr(e):
                        ins = None
                        for j in range(8):
                            kc = half * 8 + j
                            ins = e.transpose(out=pbf[:, j * 128:j * 128 + nr], in_=hb[b][0:nr, kc * 128:(kc + 1) * 128],
                                              identity=ident_bf[0:nr, 0:nr])
                        return ins
                    k.op("pe", tr, reads=[hbT[b]], writes=[pst[pb]])
                    k.op("dve", lambda e: e.tensor_tensor(
                        out=hT[:, half * 8:half * 8 + 8, row:row + nr],
                        in0=pbf.rearrange("p (j t) -> p j t", t=128)[:, :, 0:nr],
                        in1=gx[:, half * 8:half * 8 + 8, 0:nr], op=ALU.mult), reads=[pst[pb]], writes=[hTt])
                row += nr
            return hTt


        wslotT = [T() for _ in range(2)]
        wctr = [0]

        W_SPECS = ([("in", [(4096, 512)]), ("in", [(4608, 512)]), ("in", [(3072, 512)]), ("in", [(3584, 512)])]
                   + [("in", [(cc * 128, 128), (1024 + cc * 128, 128)]) for cc in range(8)]
                   + [("in", [(4096, 512)]), ("in", [(4608, 512)])]
                   + [("in", [(3072 + h * 128, 128), (2048 + h * 128, 128), (5120 + h * 128, 128)]) for h in range(8)]
                   + [("out", [(cb * 512, 512)]) for cb in range(4)])
        wissued = [0]
        wg_bf = nc.dram_tensor("wg_bf", [NE, 128, NK, DE], BF16, kind="Internal").ap()
        wu_bf = nc.dram_tensor("wu_bf", [NE, 128, NK, DE], BF16, kind="Internal").ap()
        wd_bf = nc.dram_tensor("wd_bf", [NE, 128, 4, D], BF16, kind="Internal").ap()
        pcT = [[T(), T(), T()] for _ in range(NE)]
        pcq = [(ex, j) for ex in range(NPRE) for j in range(3)]
        pci = [0]

        def precast(n):
            for _ in range(n):
                if pci[0] >= len(pcq):
                    return
                ex, j = pcq[pci[0]]
                pci[0] += 1
                srcw, dstw = ((w_gate, wg_bf), (w_up, wu_bf), (w_down, wd_bf))[j]
                if j < 2:
                    sv = srcw[ex].rearrange("(p kc) f -> p kc f", kc=NK)
                else:
                    sv = srcw[ex].rearrange("(kc p) f -> p kc f", p=128)
                if j < 2:
                    k.dma("pool", dstw[ex], sv, writes=[pcT[ex][j]], bg=True, max_dma_last_dim=4096)
                else:
                    k.dma("pool", dstw[ex], sv, writes=[pcT[ex][j]], bg=True)

        def _issue_w(j):
            which, col_ranges = W_SPECS[j]
            src_view = w_in_v if which == "in" else w_out_v
            s = j % 2
            off = 0
            for (c0, n) in col_ranges:
                k.dma("pool", wslots[s][:, :, off:off + n], src_view[:, :, c0:c0 + n], writes=[wslotT[s]])
                off += n

        def load_w(src_view, col_ranges, ahead=1):
            j = wctr[0]
            wctr[0] += 1
            assert W_SPECS[j][1] == col_ranges, (j, W_SPECS[j], col_ranges)
            while wissued[0] <= min(j + ahead, len(W_SPECS) - 1):
                _issue_w(wissued[0])
                precast(3)
                wissued[0] += 1
            return wslots[j % 2], wslotT[j % 2]

        def gates_f(st_, pf, pfT, ncol, lbh, omlh, rm, outs):
            pass

        with ExitStack() as st:
            hTp = sb("hTp", [128, NK, TPREV], BF16, st)
            hTpT = load_norm_transpose(st, x_prev, [128] * 8, hTp, gxm, "p")
            ktok = sb("ktokp", [128, 8, 8, 128], BF16, st)
            ktokT = T()
            vtok = sb("vtokp", [128, 8, 1024], BF16, st)
            vtokT = T()
            ebl = sb("eblp", [128, 8, 16], F32, st)
            eblT = T()
            tmpfA = [[sb("tmpfA%d_%d" % (q_, i), [128, 512], F32, st) for i in range(4)] for q_ in range(2)]
            tmpTA = [[T() for _ in range(4)] for q_ in range(2)]
            khatA = [sb("khatA%d" % q_, [128, 512], BF16, st) for q_ in range(2)]
            khatTA = [T() for q_ in range(2)]
            for blk in range(2):
                wsl, wT = load_w(w_in_v, [(4096 + blk * 512, 512)])
                for tt in range(8):
                    pb = 2 + (tt % 2)

                    def mmv(e):
                        ins = None
                        for kc in range(NK):
                            ins = e.matmul(ps[pb][:], lhsT=hTp[:, kc, tt * 128:(tt + 1) * 128], rhs=wsl[:, kc, :],
                                           start=(kc == 0), stop=(kc == NK - 1))
                        return ins
                    k.op("pe", mmv, reads=[hTpT, wT], writes=[pst[pb]])
                    k.op("act", lambda e: e.copy(out=vtok[:, tt, blk * 512:(blk + 1) * 512], in_=ps[pb][:]),
                         reads=[pst[pb]], writes=[vtokT])
            for hb_ in range(2):
                wsl, wT = load_w(w_in_v, [(3072 + hb_ * 512, 512)])
                for hh in range(4):
                    h = hb_ * 4 + hh
                    for nt in range(2):
                        pb = 4 + (nt % 2)

                        def mmf(e):
                            ins = None
                            for kc in range(NK):
                                ins = e.matmul(ps[pb][:], lhsT=wsl[:, kc, hh * 128:(hh + 1) * 128],
                                               rhs=hTp[:, kc, nt * 512:(nt + 1) * 512], start=(kc == 0), stop=(kc == NK - 1))
                            return ins
                        k.op("pe", mmf, reads=[hTpT, wT], writes=[pst[pb]])
                        tmpf, tmpT, khat, khatT = tmpfA[nt % 2], tmpTA[nt % 2], khatA[nt % 2], khatTA[nt % 2]
                        fs, fg, bc, em = tmpf
                        k.op("act", lambda e: e.activation(out=fs[:], in_=ps[pb][:], func=AF.Sigmoid),
                             reads=[pst[pb]], writes=[tmpT[0]])
                        k.op("dve", lambda e: e.tensor_scalar(out=fg[:], in0=fs[:], scalar1=oml[:, h:h + 1],
                                                              scalar2=lb[:, h:h + 1], op0=ALU.mult, op1=ALU.add),
                             reads=[tmpT[0]], writes=[tmpT[1]])
                        k.op("act", lambda e: e.activation(out=fs[:], in_=fg[:], func=AF.Ln),
                             reads=[tmpT[1]], writes=[tmpT[0]])
                        k.op("dve", lambda e: e.tensor_tensor_scan(out=bc[:], data0=rmask[:, 0:512], data1=fs[:],
                                                                   initial=0.0, op0=ALU.mult, op1=ALU.add),
                             reads=[tmpT[0]], writes=[tmpT[2]])
                        k.op("act", lambda e: e.activation(out=em[:], in_=bc[:], func=AF.Exp, scale=-1.0),
                             reads=[tmpT[2]], writes=[tmpT[3]])
                        k.op("act", lambda e: e.activation(
                            out=ebl[:, h, nt * 8:(nt + 1) * 8],
                            in_=bc[:].rearrange("p (c j) -> p c j", j=64)[:, :, 63], func=AF.Exp),
                            reads=[tmpT[2]], writes=[eblT])
                        k.op("dve", lambda e: e.tensor_tensor(out=fs[:], in0=fg[:], in1=em[:], op=ALU.mult),
                             reads=[tmpT[1], tmpT[3]], writes=[tmpT[0]])
                        k.op("dve", lambda e: e.tensor_tensor(out=khat[:], in0=em[:], in1=fs[:], op=ALU.subtract),
                             reads=[tmpT[0], tmpT[3]], writes=[khatT])
                        pbt = 6 + (nt % 2)
                        pbf = ps[pbt][:].bitcast(BF16)

                        def trk(e):
                            ins = None
                            for j in range(4):
                                ins = e.transpose(out=pbf[:, j * 128:(j + 1) * 128], in_=khat[:, j * 128:(j + 1) * 128],
                                                  identity=ident_bf[:])
                            return ins
                        k.op("pe", trk, reads=[khatT], writes=[pst[pbt]])
                        k.op("act", lambda e: e.copy(out=ktok[:, nt * 4:(nt + 1) * 4, h, :],
                                                     in_=pbf[:, 0:512].rearrange("p (j d) -> p j d", d=128)),
                             reads=[pst[pbt]], writes=[ktokT])
            stmp = [sb("stmpA%d" % i, [128, 128], F32, st) for i in range(2)]
            stmpT = [T() for _ in range(2)]
            for c in range(16):
                tt, r0 = c // 2, (c % 2) * 64
                for h in range(8):
                    pb = h % 4
                    k.op("pe", lambda e: e.matmul(ps[pb][:, 0:128], lhsT=ktok[r0:r0 + 64, tt, h, :],
                                                  rhs=vtok[r0:r0 + 64, tt, h * 128:(h + 1) * 128], start=True, stop=True),
                         reads=[ktokT, vtokT], writes=[pst[pb]])
                    b2 = h % 2
                    k.op("dve", lambda e: e.tensor_tensor(out=stmp[b2][:], in0=ps[pb][:, 0:128], in1=S[:, h, :], op=ALU.add),
                         reads=[pst[pb], tS[h]], writes=[stmpT[b2]])
                    k.op("dve", lambda e: e.tensor_scalar(out=S[:, h, :], in0=stmp[b2][:], scalar1=ebl[:, h, c:c + 1],
                                                          scalar2=None, op0=ALU.mult),
                         reads=[stmpT[b2], eblT], writes=[tS[h]])
            for h in range(8):
                k.op("act", lambda e: e.copy(out=S_bf[:, h, :], in_=S[:, h, :]), writes=[tS[h]])
            k.phase_end()

        if stage < 2:
            for h in range(8):
                k.dma("sp", hgrn_p[h, :, :], S[:, h, :], reads=[tS[h]])
            k.drain_dmas("sp")
            k.nc.all_engine_barrier()
            return nc

        TT = [(i * 128, 128) for i in range(8)] + [(1024, 64)]
        NT = [(0, 512), (512, 512), (1024, 64)]
        wdw = pv[:, PV_WDW:PV_WDW + 8 * CONVW]
        with ExitStack() as st:
            hT = sb("hT", [128, NK, TM], BF16, st)
            with ExitStack() as stt:
                hTt = load_norm_transpose(stt, x_main, [128] * 8 + [96], hT, gxm, "m")
                k.phase_end()
            with ExitStack() as s1:
                extc = [sb("extc%d" % i, [128, 1056], F32, s1) for i in range(2)]
                extSc = [sb("extSc%d" % i, [128, 16, 34], F32, s1) for i in range(2)]
                tails = sb("tails", [128, 8, 32], F32, s1)
                tailsT = T()
                us = sb("us", [128, 8, 64], F32, s1)
                usT = T()
                cv = sb("cv", [128, 8, TV], F32, s1)
                extT = [T() for _ in range(2)]
                extST = [T() for _ in range(2)]
                cvT = [T() for _ in range(8)]
                sct = [sb("sct%d" % i, [120, 1024], F32, s1) for i in range(4)]
                sctT = [T() for _ in range(4)]
                k.dma("sp", conv_s[:, 0:26, :], sconv[:, 4:30, :])
                for g4 in range(4):
                    k.dma("sp", sct[g4][:], sconv[g4 * 4:(g4 + 1) * 4].rearrange("b j c -> (b j) c"), writes=[sctT[g4]])
                sg = [sb("sgB%d" % i, [128, 512], F32, s1) for i in range(2)]
                sgT = [T() for _ in range(2)]
                for cc in range(8):
                    c2 = cc % 2
                    for g4 in range(4):
                        pb = g4 % 2
                        k.op("pe", lambda e: e.transpose(out=ps[pb][:, 0:120], in_=sct[g4][:, cc * 128:(cc + 1) * 128],
                                                         identity=ident_f[0:120, 0:120]), reads=[sctT[g4]], writes=[pst[pb]])
                        k.op("act", lambda e: e.copy(out=extSc[c2][:, g4 * 4:(g4 + 1) * 4, 0:30],
                                                     in_=ps[pb][:, 0:120].rearrange("p (b j) -> p b j", j=30)),
                             reads=[pst[pb]], writes=[extST[c2]])
                    wsl, wT = load_w(w_in_v, [(cc * 128, 128), (1024 + cc * 128, 128)])
                    for nt, (t0, n) in enumerate([(0, 512), (512, 512), (1024, 96)]):
                        pa = 2 + (nt % 2) * 2
                        pg = pa + 1

                        def mma(e):
                            ins = None
                            for kc in range(NK):
                                ins = e.matmul(ps[pa][:, 0:n], lhsT=wsl[:, kc, 0:128], rhs=hT[:, kc, t0:t0 + n],
                                               start=(kc == 0), stop=(kc == NK - 1))
                            return ins

                        def mmg(e):
                            ins = None
                            for kc in range(NK):
                                ins = e.matmul(ps[pg][:, 0:n], lhsT=wsl[:, kc, 128:256], rhs=hT[:, kc, t0:t0 + n],
                                               start=(kc == 0), stop=(kc == NK - 1))
                            return ins
                        k.op("pe", mma, reads=[hTt, wT], writes=[pst[pa]])
                        k.op("pe", mmg, reads=[hTt, wT], writes=[pst[pg]])
                        s_ = nt % 2
                        k.op("act", lambda e: e.activation(out=sg[s_][:, 0:n], in_=ps[pg][:, 0:n], func=AF.Sigmoid),
                             reads=[pst[pg]], writes=[sgT[s_]])
                        if nt < 2:
                            k.op("dve", lambda e: e.tensor_tensor(out=extc[c2][:, 32 + t0:32 + t0 + n], in0=ps[pa][:, 0:n],
                                                                  in1=sg[s_][:, 0:n], op=ALU.mult),
                                 reads=[pst[pa], sgT[s_]], writes=[extT[c2]])
                        else:
                            k.op("dve", lambda e: e.tensor_tensor(
                                out=extSc[c2][:, :, 30:34], in0=ps[pa][:, 0:64].rearrange("p (b t) -> p b t", t=4),
                                in1=sg[s_][:, 0:64].rearrange("p (b t) -> p b t", t=4), op=ALU.mult),
                                reads=[pst[pa], sgT[s_]], writes=[extST[c2]])
                            k.op("dve", lambda e: e.tensor_tensor(out=extc[c2][:, 0:32], in0=ps[pa][:, 64:96],
                                                                  in1=sg[s_][:, 64:96], op=ALU.mult),
                                 reads=[pst[pa], sgT[s_]], writes=[extT[c2]])
                    k.op("dve", lambda e: e.tensor_scalar(out=cv[:, cc, 0:TCUR], in0=extc[c2][:, 2:2 + TCUR],
                                                          scalar1=wdw[:, cc * CONVW:cc * CONVW + 1],
                                                          scalar2=pv[:, PV_BDW + cc:PV_BDW + cc + 1], op0=ALU.mult, op1=ALU.add),
                         reads=[extT[c2]], writes=[cvT[cc]])
                    for j in range(1, CONVW):
                        k.op("dve", lambda e: e.scalar_tensor_tensor(out=cv[:, cc, 0:TCUR], in0=extc[c2][:, 2 + j:2 + j + TCUR],
                                                                     scalar=wdw[:, cc * CONVW + j:cc * CONVW + j + 1],
                                                                     in1=cv[:, cc, 0:TCUR], op0=ALU.mult, op1=ALU.add),
                             reads=[extT[c2]], writes=[cvT[cc]])
                    cvs = cv[:, cc, TCUR:TV].rearrange("p (b t) -> p b t", t=4)
                    k.op("dve", lambda e: e.tensor_scalar(out=cvs, in0=extSc[c2][:, :, 0:4],
                                                          scalar1=wdw[:, cc * CONVW:cc * CONVW + 1],
                                                          scalar2=pv[:, PV_BDW + cc:PV_BDW + cc + 1], op0=ALU.mult, op1=ALU.add),
                         reads=[extST[c2]], writes=[cvT[cc]])
                    for j in range(1, CONVW):
                        k.op("dve", lambda e: e.scalar_tensor_tensor(out=cvs, in0=extSc[c2][:, :, j:j + 4],
                                                                     scalar=wdw[:, cc * CONVW + j:cc * CONVW + j + 1],
                                                                     in1=cvs, op0=ALU.mult, op1=ALU.add),
                             reads=[extST[c2]], writes=[cvT[cc]])
                    k.op("act", lambda e: e.copy(out=tails[:, cc, :], in_=extc[c2][:, 1024:1056]), reads=[extT[c2]], writes=[tailsT])
                    k.op("act", lambda e: e.copy(out=us[:, cc, :].rearrange("p (b t) -> p b t", t=4), in_=extSc[c2][:, :, 30:34]),
                         reads=[extST[c2]], writes=[usT])
                cpo = sb("cpo", [64, 1024], F32, s1)
                cpoT = T()
                for half in range(2):
                    pb = half

                    def trc(e):
                        ins = None
                        for j in range(4):
                            cc = half * 4 + j
                            ins = e.transpose(out=ps[pb][0:32, j * 128:(j + 1) * 128], in_=tails[:, cc, :],
                                              identity=ident_f[:])
                        return ins
                    k.op("pe", trc, reads=[tailsT], writes=[pst[pb]])
                    k.op("act", lambda e: e.copy(out=cpo[0:32, half * 512:(half + 1) * 512], in_=ps[pb][0:32, :]),
                         reads=[pst[pb]], writes=[cpoT])
                k.dma("sp", conv_p[:, :], cpo[2:32, :], reads=[cpoT])
                cso = cpo
                csoT = cpoT
                for half in range(2):
                    pb = 2 + half

                    def trs(e):
                        ins = None
                        for j in range(4):
                            cc = half * 4 + j
                            ins = e.transpose(out=ps[pb][0:64, j * 128:(j + 1) * 128], in_=us[:, cc, :], identity=ident_f[:])
                        return ins
                    k.op("pe", trs, reads=[usT], writes=[pst[pb]])
                    k.op("act", lambda e: e.copy(out=cso[:, half * 512:(half + 1) * 512], in_=ps[pb][0:64, :]),
                         reads=[pst[pb]], writes=[csoT])
                for b in range(16):
                    k.dma("sp", conv_s[b, 26:30, :], cso[b * 4:(b + 1) * 4, :], reads=[csoT])
                sq = [sb("sqB%d" % i, [128, 512], F32, s1) for i in range(2)]
                sqT = [T() for _ in range(2)]
                mean = sb("meanB", [128, 512], F32, s1)
                rstd = sb("rstdB", [128, 512], F32, s1)
                msq = sb("msqB", [128, 512], F32, s1)
                stT = T()
                t1 = sg
                t1T = sgT
                for (t0, n) in NT:
                    for cc in range(8):
                        s_ = cc % 2
                        k.op("act", lambda e: e.activation(out=sq[s_][:, 0:n], in_=cv[:, cc, t0:t0 + n], func=AF.Square),
                             reads=[cvT[cc]], writes=[sqT[s_]])
                        k.op("pe", lambda e: e.matmul(ps[6][:, 0:n], lhsT=ones_f[:], rhs=cv[:, cc, t0:t0 + n],
                                                      start=(cc == 0), stop=(cc == 7)), reads=[cvT[cc]], writes=[pst[6]])
                        k.op("pe", lambda e: e.matmul(ps[7][:, 0:n], lhsT=ones_f[:], rhs=sq[s_][:, 0:n],
                                                      start=(cc == 0), stop=(cc == 7)), reads=[sqT[s_]], writes=[pst[7]])
                    k.op("dve", lambda e: e.tensor_scalar(out=mean[:, 0:n], in0=ps[6][:, 0:n], scalar1=1.0 / 1024, scalar2=None,
                                                          op0=ALU.mult), reads=[pst[6]], writes=[stT])
                    k.op("dve", lambda e: e.tensor_tensor(out=msq[:, 0:n], in0=mean[:, 0:n], in1=mean[:, 0:n], op=ALU.mult),
                         writes=[stT])
                    k.op("dve", lambda e: e.scalar_tensor_tensor(out=msq[:, 0:n], in0=ps[7][:, 0:n], scalar=1.0 / 1024,
                                                                 in1=msq[:, 0:n], op0=ALU.mult, op1=ALU.subtract),
                         reads=[pst[7]], writes=[stT])
                    k.op("act", lambda e: e.activation(out=msq[:, 0:n], in_=msq[:, 0:n], func=AF.Sqrt, bias=pv_eps[:, :]),
                         writes=[stT])
                    k.op("dve", lambda e: e.reciprocal(out=rstd[:, 0:n], in_=msq[:, 0:n]), writes=[stT])
                    for cc in range(8):
                        s_ = cc % 2
                        k.op("dve", lambda e: e.tensor_tensor(out=t1[s_][:, 0:n], in0=cv[:, cc, t0:t0 + n], in1=mean[:, 0:n],
                                                              op=ALU.subtract), reads=[cvT[cc], stT], writes=[t1T[s_]])
                        k.op("dve", lambda e: e.tensor_tensor(out=t1[s_][:, 0:n], in0=t1[s_][:, 0:n], in1=rstd[:, 0:n],
                                                              op=ALU.mult), reads=[stT], writes=[t1T[s_]])
                        k.op("act", lambda e: e.activation(out=cat[:, cc, t0:t0 + n], in_=t1[s_][:, 0:n], func=AF.Silu,
                                                           scale=pv[:, PV_LNG + cc:PV_LNG + cc + 1],
                                                           bias=pv[:, PV_LNB + cc:PV_LNB + cc + 1]),
                             reads=[t1T[s_]], writes=[catTs[cc]])
                k.phase_end()
            if stage < 3:
                es.pop_all()
                return nc
            with ExitStack() as s2:
                vtok = sb("vtok", [128, 9, 1024], BF16, s2)
                vtokT = T()
                for blk in range(2):
                    wsl, wT = load_w(w_in_v, [(4096 + blk * 512, 512)])
                    for tt, (r0, nr) in enumerate(TT):
                        pb = tt % 2

                        def mmv(e):
                            ins = None
                            for kc in range(NK):
                                ins = e.matmul(ps[pb][0:nr, :], lhsT=hT[:, kc, r0:r0 + nr], rhs=wsl[:, kc, :],
                                               start=(kc == 0), stop=(kc == NK - 1))
                            return ins
                        k.op("pe", mmv, reads=[hTt, wT], writes=[pst[pb]])
                        k.op("act", lambda e: e.copy(out=vtok[0:nr, tt, blk * 512:(blk + 1) * 512], in_=ps[pb][0:nr, :]),
                             reads=[pst[pb]], writes=[vtokT])
                qtS = sb("qtS", [128, 8, 64], BF16, s2)
                sggS = sb("sggS", [128, 8, 64], BF16, s2)
                attmS = sb("attmS", [64, 8, 64], BF16, s2)
                qtST, sggST, attmST = T(), T(), T()
                zer = sb("zer", [128, 128], BF16, s2)
                ind = sb("ind", [64, 16], F32, s2)
                indT = T()
                k.op("pool", lambda e: e.memset(zer[:], 0.0), writes=[indT])
                k.op("pool", lambda e: e.memset(ind[:], 1.0), writes=[indT])
                k.op("pool", lambda e: e.affine_select(out=ind[:], in_=ind[:], pattern=[[-4, 16]], compare_op=ALU.is_ge,
                                                       fill=0.0, base=0, channel_multiplier=1), writes=[indT])
                k.op("pool", lambda e: e.affine_select(out=ind[:], in_=ind[:], pattern=[[4, 16]], compare_op=ALU.is_ge,
                                                       fill=0.0, base=3, channel_multiplier=-1), writes=[indT])
                ktokS = sb("ktokS", [64, 8, 128], BF16, s2)
                ktokST = T()
                ebS = sb("ebS", [128, 8, 16], F32, s2)
                ebST = T()
                gn = pv[:, PV_GN:PV_GN + 1]
                sH = ExitStack()
                R2 = []
                for p in range(2):
                    r = {}
                    r["khat"] = sb("khatB%d" % p, [128, TV], BF16, sH)
                    r["qt"] = sb("qtB%d" % p, [128, TV], BF16, sH)
                    r["sgg"] = sb("sggB%d" % p, [128, TV], BF16, sH)
                    r["ktok"] = sb("ktokB%d" % p, [128, 8, 128], BF16, sH)
                    r["ebl"] = sb("eblB%d" % p, [128, 16], F32, sH)
                    r["tmpf"] = [sb("tmpfB%d_%d" % (p, i), [128, 512], F32, sH) for i in range(5)]
                    r["attm"] = sb("attm%d" % p, [128, 128], BF16, sH)
                    r["osq"] = sb("osq%d" % p, [128, 128], F32, sH)
                    r["orr"] = sb("orr%d" % p, [128, 128], F32, sH)
                    r["stmp"] = sb("stmpB%d" % p, [128, 128], F32, sH)
                    for nm in ("khatT", "qtT", "sggT", "ktokT", "eblT", "attmT", "osqT", "orrT", "stmpT"):
                        r[nm] = T()
                    r["tmpT"] = [T() for _ in range(5)]
                    R2.append(r)

                def head_gen(h):
                    p = h % 2
                    r = R2[p]
                    khat, qt, sgg, ktok, ebl, tmpf, attm, osq, orr, stmp = (r["khat"], r["qt"], r["sgg"], r["ktok"], r["ebl"],
                                                                           r["tmpf"], r["attm"], r["osq"], r["orr"], r["stmp"])
                    khatT, qtT, sggT, ktokT, eblT, attmT, osqT, orrT, stmpT, tmpT = (r["khatT"], r["qtT"], r["sggT"], r["ktokT"],
                                                                                     r["eblT"], r["attmT"], r["osqT"], r["orrT"],
                                                                                     r["stmpT"], r["tmpT"])
                    B0 = 4 * p
                    wsl, wT = load_w(w_in_v, [(3072 + h * 128, 128), (2048 + h * 128, 128), (5120 + h * 128, 128)], ahead=0)
                    for nt, (t0, n) in enumerate(NT):
                        for ci in range(3):
                            pb = B0 + ci

                            def mmx(e):
                                ins = None
                                for kc in range(NK):
                                    ins = e.matmul(ps[pb][:, 0:n], lhsT=wsl[:, kc, ci * 128:(ci + 1) * 128],
                                                   rhs=hT[:, kc, t0:t0 + n], start=(kc == 0), stop=(kc == NK - 1))
                                return ins
                            k.op("pe", mmx, reads=[hTt, wT], writes=[pst[pb]])
                        yield
                        fs, fg, bc, em, qs = tmpf
                        k.op("act", lambda e: e.activation(out=fs[:, 0:n], in_=ps[B0][:, 0:n], func=AF.Sigmoid),
                             reads=[pst[B0]], writes=[tmpT[0]])
                        k.op("act", lambda e: e.activation(out=qs[:, 0:n], in_=ps[B0 + 1][:, 0:n], func=AF.Silu),
                             reads=[pst[B0 + 1]], writes=[tmpT[4]])
                        k.op("act", lambda e: e.activation(out=sgg[:, t0:t0 + n], in_=ps[B0 + 2][:, 0:n], func=AF.Silu),
                             reads=[pst[B0 + 2]], writes=[sggT])
                        k.op("dve", lambda e: e.tensor_scalar(out=fg[:, 0:n], in0=fs[:, 0:n], scalar1=oml[:, h:h + 1],
                                                              scalar2=lb[:, h:h + 1], op0=ALU.mult, op1=ALU.add),
                             reads=[tmpT[0]], writes=[tmpT[1]])
                        k.op("act", lambda e: e.activation(out=fs[:, 0:n], in_=fg[:, 0:n], func=AF.Ln),
                             reads=[tmpT[1]], writes=[tmpT[0]])
                        k.op("dve", lambda e: e.tensor_tensor_scan(out=bc[:, 0:n], data0=rmask[:, t0:t0 + n], data1=fs[:, 0:n],
                                                                   initial=0.0, op0=ALU.mult, op1=ALU.add),
                             reads=[tmpT[0]], writes=[tmpT[2]])
                        yield
                        k.op("act", lambda e: e.activation(out=em[:, 0:n], in_=bc[:, 0:n], func=AF.Exp, scale=-1.0),
                             reads=[tmpT[2]], writes=[tmpT[3]])
                        k.op("act", lambda e: e.activation(out=fs[:, 0:n], in_=bc[:, 0:n], func=AF.Exp),
                             reads=[tmpT[2]], writes=[tmpT[0]])
                        if nt < 2:
                            k.op("act", lambda e: e.copy(out=ebl[:, nt * 8:(nt + 1) * 8],
                                                         in_=fs[:, 0:n].rearrange("p (c j) -> p c j", j=64)[:, :, 63]),
                                 reads=[tmpT[0]], writes=[eblT])
                        else:
                            k.op("act", lambda e: e.copy(out=ebS[:, h, :],
                                                         in_=fs[:, 0:n].rearrange("p (c j) -> p c j", j=4)[:, :, 3]),
                                 reads=[tmpT[0]], writes=[ebST])
                        k.op("dve", lambda e: e.tensor_tensor(out=qt[:, t0:t0 + n], in0=qs[:, 0:n], in1=fs[:, 0:n], op=ALU.mult),
                             reads=[tmpT[4], tmpT[0]], writes=[qtT])
                        k.op("dve", lambda e: e.tensor_tensor(out=fg[:, 0:n], in0=fg[:, 0:n], in1=em[:, 0:n], op=ALU.mult),
                             reads=[tmpT[3]], writes=[tmpT[1]])
                        k.op("dve", lambda e: e.tensor_tensor(out=khat[:, t0:t0 + n], in0=em[:, 0:n], in1=fg[:, 0:n],
                                                              op=ALU.subtract), reads=[tmpT[1], tmpT[3]], writes=[khatT])
                        yield
                    pbt = B0 + 3
                    pbf = ps[pbt][:].bitcast(BF16)
                    for g2 in range(2):
                        def trk(e):
                            ins = None
                            for j in range(4):
                                tt = g2 * 4 + j
                                ins = e.transpose(out=pbf[:, j * 128:(j + 1) * 128], in_=khat[:, tt * 128:(tt + 1) * 128],
                                                  identity=ident_bf[:])
                            return ins
                        k.op("pe", trk, reads=[khatT], writes=[pst[pbt]])
                        k.op("act", lambda e: e.copy(out=ktok[:, g2 * 4:(g2 + 1) * 4, :],
                                                     in_=pbf[:, 0:512].rearrange("p (j d) -> p j d", d=128)),
                             reads=[pst[pbt]], writes=[ktokT])
                    k.op("pe", lambda e: e.transpose(out=pbf[0:64, 0:128], in_=khat[:, 1024:1088], identity=ident_bf[:]),
                         reads=[khatT], writes=[pst[pbt]])
                    k.op("act", lambda e: e.copy(out=ktokS[:, h, :], in_=pbf[0:64, 0:128]), reads=[pst[pbt]], writes=[ktokST])
                    yield
                    pA, pO, pS, pQ = B0, B0 + 1, B0 + 2, B0 + 3
                    for pp in range(8):
                        c0 = pp * 128
                        k.op("pe", lambda e: e.matmul(ps[pA][:, 0:128], lhsT=khat[:, c0:c0 + 128], rhs=qt[:, c0:c0 + 128],
                                                      start=True, stop=True), reads=[khatT, qtT], writes=[pst[pA]])
                        k.op("dve", lambda e: e.tensor_tensor(out=attm[:], in0=ps[pA][:, 0:128], in1=maskP[:], op=ALU.mult),
                             reads=[pst[pA]], writes=[attmT])
                        k.op("pe", lambda e: e.matmul(ps[pO][:, 0:128], lhsT=vtok[:, pp, h * 128:(h + 1) * 128], rhs=attm[:],
                                                      start=True, stop=False), reads=[vtokT, attmT], writes=[pst[pO]])
                        yield
                        for sub in range(2):
                            cs = c0 + sub * 64
                            r0 = sub * 64
                            k.op("pe", lambda e: e.matmul(ps[pO][:, r0:r0 + 64], lhsT=S_bf[:, h, :], rhs=qt[:, cs:cs + 64],
                                                          start=False, stop=(sub == 1)), reads=[tS[h], qtT], writes=[pst[pO]])
                            k.op("pe", lambda e: e.matmul(ps[pS][:, 0:128], lhsT=ktok[r0:r0 + 64, pp, :],
                                                          rhs=vtok[r0:r0 + 64, pp, h * 128:(h + 1) * 128], start=True, stop=True),
                                 reads=[ktokT, vtokT], writes=[pst[pS]])
                            k.op("dve", lambda e: e.tensor_tensor(out=stmp[:], in0=ps[pS][:, 0:128], in1=S[:, h, :], op=ALU.add),
                                 reads=[pst[pS], tS[h]], writes=[stmpT])
                            ci_ = pp * 2 + sub
                            k.op("dve", lambda e: e.tensor_scalar(out=S[:, h, :], in0=stmp[:], scalar1=ebl[:, ci_:ci_ + 1],
                                                                  scalar2=None, op0=ALU.mult),
                                 reads=[stmpT, eblT], writes=[tS[h]])
                            k.op("act", lambda e: e.copy(out=S_bf[:, h, :], in_=S[:, h, :]), writes=[tS[h]])
                            yield
                        n = 128
                        k.op("act", lambda e: e.activation(out=osq[:, 0:n], in_=ps[pO][:, 0:n], func=AF.Square),
                             reads=[pst[pO]], writes=[osqT])
                        k.op("pe", lambda e: e.matmul(ps[pQ][:, 0:n], lhsT=ones_f[:], rhs=osq[:, 0:n], start=True, stop=True),
                             reads=[osqT], writes=[pst[pQ]])
                        k.op("act", lambda e: e.activation(out=orr[:, 0:n], in_=ps[pQ][:, 0:n], func=AF.Sqrt, scale=1.0 / 128,
                                                           bias=pv_eps[:, :]), reads=[pst[pQ]], writes=[orrT])
                        yield
                        k.op("dve", lambda e: e.reciprocal(out=orr[:, 0:n], in_=orr[:, 0:n]), writes=[orrT])
                        k.op("dve", lambda e: e.tensor_tensor(out=orr[:, 0:n], in0=ps[pO][:, 0:n], in1=orr[:, 0:n], op=ALU.mult),
                             reads=[pst[pO]], writes=[orrT])
                        k.op("dve", lambda e: e.scalar_tensor_tensor(out=cat[:, 8 + h, c0:c0 + n], in0=orr[:, 0:n], scalar=gn,
                                                                     in1=sgg[:, c0:c0 + n], op0=ALU.mult, op1=ALU.mult),
                             reads=[orrT, sggT], writes=[catTs[8 + h]])
                        yield
                    k.op("pe", lambda e: e.matmul(ps[pA][0:64, 0:64], lhsT=khat[:, 1024:1088], rhs=qt[:, 1024:1088],
                                                  start=True, stop=True), reads=[khatT, qtT], writes=[pst[pA]])
                    k.op("dve", lambda e: e.tensor_tensor(out=attmS[:, h, :], in0=ps[pA][0:64, 0:64], in1=maskS[:], op=ALU.mult),
                         reads=[pst[pA]], writes=[attmST])
                    k.op("act", lambda e: e.copy(out=qtS[:, h, :], in_=qt[:, 1024:1088]), reads=[qtT], writes=[qtST])
                    k.op("act", lambda e: e.copy(out=sggS[:, h, :], in_=sgg[:, 1024:1088]), reads=[sggT], writes=[sggST])

                gens = [head_gen(h) for h in range(8)]
                active = [gens[0], gens[1]]
                nxt = 2
                for _ in range(6):
                    next(active[0])
                while active:
                    for g in list(active):
                        try:
                            next(g)
                        except StopIteration:
                            active.remove(g)
                            if nxt < 8:
                                active.append(gens[nxt])
                                nxt += 1
                k.soft_barrier()
                sH.close()
                k.dma("sp", hgrn_p.rearrange("h d v -> d h v"), S[:], reads=tS)
                def mm0(e):
                    e.matmul(ps[5][:, :], lhsT=zer[:], rhs=hT[:, 0, 0:512], start=True, stop=False)
                    ins = None
                    for h in range(8):
                        ins = e.matmul(ps[5][:, h * 64:(h + 1) * 64], lhsT=vtok[0:64, 8, h * 128:(h + 1) * 128],
                                       rhs=attmS[:, h, :], start=False, stop=False)
                    return ins
                k.op("pe", mm0, reads=[indT, vtokT, attmST, hTt], writes=[pst[5]])
                Sf = [sb("Sf%d" % i, [128, 8, 128], F32, s2) for i in range(2)]
                SfT = [T() for _ in range(2)]
                Sbb = [sb("Sbb%d" % i, [128, 8, 128], BF16, s2) for i in range(2)]
                SbbT = [T() for _ in range(2)]
                vm = [sb("vm%d" % i, [64, 1024], BF16, s2) for i in range(2)]
                vmT = [T() for _ in range(2)]
                stm2 = sb("stm2", [128, 8, 128], F32, s2)
                stm2T = T()
                for b in range(16):
                    b2 = b % 2
                    k.dma("sp", Sf[b2][:], shgrn[b].rearrange("h d v -> d h v"), writes=[SfT[b2]])
                    k.dma("pool", Sbb[b2][:], shgrn[b].rearrange("h d v -> d h v"), writes=[SbbT[b2]])

                    def mmi(e):
                        ins = None
                        for h in range(8):
                            ins = e.matmul(ps[5][:, h * 64 + 4 * b:h * 64 + 4 * b + 4], lhsT=Sbb[b2][:, h, :],
                                           rhs=qtS[:, h, 4 * b:4 * b + 4], start=False, stop=(b == 15 and h == 7))
                        return ins
                    k.op("pe", mmi, reads=[SbbT[b2], qtST], writes=[pst[5]])
                    k.op("dve", lambda e: e.tensor_scalar(out=vm[b2][:], in0=vtok[0:64, 8, :], scalar1=ind[:, b:b + 1],
                                                          scalar2=None, op0=ALU.mult), reads=[vtokT, indT], writes=[vmT[b2]])
                    for half in range(2):
                        pb = 2 + half

                        def mmu(e):
                            ins = None
                            for j in range(4):
                                h = half * 4 + j
                                ins = e.matmul(ps[pb][:, j * 128:(j + 1) * 128], lhsT=ktokS[:, h, :],
                                               rhs=vm[b2][:, h * 128:(h + 1) * 128], start=True, stop=True)
                            return ins
                        k.op("pe", mmu, reads=[ktokST, vmT[b2]], writes=[pst[pb]])
                        k.op("dve", lambda e: e.tensor_tensor(out=stm2[:, half * 4:half * 4 + 4, :],
                                                              in0=ps[pb][:].rearrange("p (j v) -> p j v", v=128),
                                                              in1=Sf[b2][:, half * 4:half * 4 + 4, :], op=ALU.add),
                             reads=[pst[pb], SfT[b2]], writes=[stm2T])
                    k.op("dve", lambda e: e.tensor_tensor(out=Sf[b2][:], in0=stm2[:],
                                                          in1=ebS[:, :, b:b + 1].to_broadcast([128, 8, 128]), op=ALU.mult),
                         reads=[stm2T, ebST], writes=[SfT[b2]])
                    k.dma("sp", hgrn_s[b].rearrange("h d v -> d h v"), Sf[b2][:], reads=[SfT[b2]])
                osqw = sb("osqw", [128, 512], F32, s2)
                orrw = sb("orrw", [128, 512], F32, s2)
                tmpT = [T(), T()]
                k.op("act", lambda e: e.activation(out=osqw[:], in_=ps[5][:], func=AF.Square), reads=[pst[5]], writes=[tmpT[0]])
                k.op("pe", lambda e: e.matmul(ps[7][:], lhsT=ones_f[:], rhs=osqw[:], start=True, stop=True),
                     reads=[tmpT[0]], writes=[pst[7]])
                k.op("act", lambda e: e.activation(out=orrw[:], in_=ps[7][:], func=AF.Sqrt, scale=1.0 / 128, bias=pv_eps[:, :]),
                     reads=[pst[7]], writes=[tmpT[1]])
                k.op("dve", lambda e: e.reciprocal(out=orrw[:], in_=orrw[:]), writes=[tmpT[1]])
                k.op("dve", lambda e: e.tensor_tensor(out=orrw[:], in0=ps[5][:], in1=orrw[:], op=ALU.mult),
                     reads=[pst[5]], writes=[tmpT[1]])
                k.op("dve", lambda e: e.scalar_tensor_tensor(out=cat[:, 8:16, 1024:1088],
                                                             in0=orrw[:].rearrange("p (h t) -> p h t", t=64), scalar=gn,
                                                             in1=sggS[:], op0=ALU.mult, op1=ALU.mult),
                     reads=[tmpT[1], sggST], writes=catTs[8:16])
                k.phase_end()
        stM.close()
        stC = ExitStack()
        acc = sb("acc", [128, 9, D], F32, stC)
        accT = [T() for _ in range(9)]
        comb = sb("comb", [128, 9, 32], F32, stC)
        combT = T()
        wk = sb("wk", [128, 9, 2], F32, stC)
        sloti = sb("sloti", [128, 9, 2], I32, stC)
        flagi = sb("flagi", [1, 2], I32, stC)
        dispT = T()
        h2T, h2Tt = cat, catT
        with ExitStack() as s3:
            gfr = sb("gfr", [128, D], F32, s3)
            gfrT = T()
            k.dma("sp", gfr[:], gffn_rep[:, :], writes=[gfrT])
            hbs = [sb("hbs%d" % i, [128, D], BF16, s3) for i in range(9)]
            hbsT = [T() for _ in range(9)]
            k.op("pool", lambda e: e.memset(hbs[8][64:128, :], 0.0), writes=[hbsT[8]])
            xres = [sb("xres%d" % i, [128, 512], F32, s3) for i in range(2)]
            xresT = [T() for _ in range(2)]
            wr = sb("wr", [128, NK, 36], BF16, s3)
            wrT = T()
            brt = sb("brt", [128, 36], F32, s3)
            k.dma("pool", wr[:], w_r_v, writes=[wrT])
            k.dma("sp", brt[:], b_r[:, :], writes=[wrT])
            for cb in range(4):
                wsl, wT = load_w(w_out_v, [(cb * 512, 512)])
                for tt, (r0, nr) in enumerate(TT):
                    b2 = tt % 2
                    pb = tt % 4
                    k.dma("sp", xres[b2][0:nr, :], x_main[r0:r0 + nr, cb * 512:(cb + 1) * 512], writes=[xresT[b2]])

                    def mmo(e):
                        ins = None
                        for kc in range(NK):
                            ins = e.matmul(ps[pb][0:nr, :], lhsT=cat[:, kc, r0:r0 + nr], rhs=wsl[:, kc, :],
                                           start=(kc == 0), stop=(kc == NK - 1))
                        return ins
                    k.op("pe", mmo, reads=catTs + [catT, wT], writes=[pst[pb]])
                    k.op("dve", lambda e: e.tensor_tensor(out=acc[0:nr, tt, cb * 512:(cb + 1) * 512], in0=ps[pb][0:nr, :],
                                                          in1=xres[b2][0:nr, :], op=ALU.add),
                         reads=[pst[pb], xresT[b2]], writes=[accT[tt]])
            Uu = sb("Uu", [128, 128], BF16, s3)
            ones_bf = sb("ones_bf", [128, 128], BF16, s3)
            base32 = sb("base32", [128, 32], F32, s3)
            ohA = sb("ohA", [128, 9, 32], F32, s3)
            ohB = sb("ohB", [128, 9, 32], F32, s3)
            sel = sb("sel", [128, 9, 32], BF16, s3)
            slotf = sb("slotf", [128, 9, 2], F32, s3)
            dcT = T()
            k.op("pool", lambda e: e.memset(Uu[:], 1.0), writes=[dcT])
            k.op("pool", lambda e: e.affine_select(out=Uu[:], in_=Uu[:], pattern=[[1, 128]], compare_op=ALU.is_gt, fill=0.0,
                                                   base=0, channel_multiplier=-1), writes=[dcT])
            k.op("pool", lambda e: e.memset(ones_bf[:], 1.0), writes=[dcT])
            k.op("pool", lambda e: e.iota(base32[:], pattern=[[CAP, 32]], base=0, channel_multiplier=0,
                                          allow_small_or_imprecise_dtypes=True), writes=[dcT])
            k.op("pool", lambda e: e.memset(sel[:], 0.0), writes=[dcT])
            k.op("pool", lambda e: e.memset(ohA[:], 0.0), writes=[dcT])
            k.op("pool", lambda e: e.memset(ohB[:], 0.0), writes=[dcT])
            k.op("pool", lambda e: e.memset(slotf[:], BIGSLOT), writes=[dcT])
            k.op("pool", lambda e: e.memset(wk[:], 0.0), writes=[dcT])
            junk = sb("junkC", [128, D], BF16, s3)
            junkT = T()
            ss = sb("ssC", [128, 4], F32, s3)
            ssT = T()
            for tt, (r0, nr) in enumerate(TT):
                P = slice(0, nr)
                k.op("act", lambda e: e.activation(out=junk[P, :], in_=acc[P, tt, :], func=AF.Square, accum_out=ss[P, 0:1]),
                     reads=[accT[tt]], writes=[junkT, ssT])
                k.op("act", lambda e: e.activation(out=ss[P, 1:2], in_=ss[P, 0:1], func=AF.Sqrt, scale=1.0 / D, bias=pv_eps[P, :]),
                     writes=[ssT])
                k.op("dve", lambda e: e.reciprocal(out=ss[P, 2:3], in_=ss[P, 1:2]), writes=[ssT])
                k.op("dve", lambda e: e.scalar_tensor_tensor(out=hbs[tt][P, :], in0=acc[P, tt, :], scalar=ss[P, 2:3], in1=gfr[P, :],
                                                             op0=ALU.mult, op1=ALU.mult),
                     reads=[accT[tt], ssT, gfrT], writes=[hbsT[tt]])
                for half in range(2):
                    pb = half
                    pbf = ps[pb][:].bitcast(BF16)

                    def tr(e):
                        ins = None
                        for j in range(8):
                            kc = half * 8 + j
                            ins = e.transpose(out=pbf[:, j * 128:j * 128 + nr], in_=hbs[tt][P, kc::NK],
                                              identity=ident_bf[P, P])
                        return ins
                    k.op("pe", tr, reads=[hbsT[tt]], writes=[pst[pb]])
                    k.op("act", lambda e: e.copy(out=h2T[:, half * 8:half * 8 + 8, r0:r0 + nr],
                                                 in_=pbf.rearrange("p (j t) -> p j t", t=128)[:, :, 0:nr]),
                         reads=[pst[pb]], writes=[h2Tt])

                def mmr(e):
                    ins = None
                    for kc in range(NK):
                        ins = e.matmul(ps[4][P, tt * 36:(tt + 1) * 36], lhsT=h2T[:, kc, r0:r0 + nr], rhs=wr[:, kc, :],
                                       start=(kc == 0), stop=(kc == NK - 1))
                    return ins
                k.op("pe", mmr, reads=[h2Tt, wrT], writes=[pst[4]])
            lgA = sb("lgA", [128, 9, 36], F32, s3)
            r9 = sb("r9", [128, 12, 9], F32, s3)
            w4 = sb("w4", [128, 2, 9, 4], F32, s3)
            w8 = sb("w8", [128, 5, 9, 8], F32, s3)
            rtT = T()

            def R(fn, e="dve", reads=()):
                k.op(e, fn, reads=list(reads), writes=[rtT])

            def bc(ap2, w):
                return ap2.unsqueeze(2).to_broadcast([128, 9, w])
            gmax, gsum, ptop, m1, m2, dd, ed, den = (r9[:, i, :] for i in range(8))
            ohg, ge = w4[:, 0], w4[:, 1]
            sel8, oh1, msk, oh2, t8 = (w8[:, i] for i in range(5))
            R(lambda e: e.tensor_tensor(out=lgA[:], in0=ps[4][:, 0:324].rearrange("p (a b) -> p a b", b=36),
                                        in1=brt[:].unsqueeze(1).to_broadcast([128, 9, 36]), op=ALU.add), reads=[pst[4], wrT])
            R(lambda e: e.tensor_reduce(out=gmax, in_=lgA[:, :, 0:4], axis=AX.X, op=ALU.max))
            R(lambda e: e.tensor_tensor(out=ohg, in0=lgA[:, :, 0:4], in1=bc(gmax, 4), op=ALU.is_equal))
            R(lambda e: e.tensor_tensor(out=ge, in0=lgA[:, :, 0:4], in1=bc(gmax, 4), op=ALU.subtract))
            R(lambda e: e.activation(out=ge, in_=ge, func=AF.Exp), "act")
            R(lambda e: e.tensor_reduce(out=gsum, in_=ge, axis=AX.X, op=ALU.add))
            R(lambda e: e.reciprocal(out=ptop, in_=gsum))
            R(lambda e: e.tensor_tensor(out=sel8, in0=lgA[:, :, 4:12], in1=bc(w4[:, 0, :, 0], 8), op=ALU.mult))
            for g in range(1, 4):
                R(lambda e: e.tensor_tensor(out=t8, in0=lgA[:, :, 4 + 8 * g:12 + 8 * g], in1=bc(w4[:, 0, :, g], 8), op=ALU.mult))
                R(lambda e: e.tensor_tensor(out=sel8, in0=sel8, in1=t8, op=ALU.add))
            R(lambda e: e.tensor_reduce(out=m1, in_=sel8, axis=AX.X, op=ALU.max))
            R(lambda e: e.tensor_tensor(out=oh1, in0=sel8, in1=bc(m1, 8), op=ALU.is_equal))
            R(lambda e: e.scalar_tensor_tensor(out=msk, in0=oh1, scalar=-1e30, in1=sel8, op0=ALU.mult, op1=ALU.add))
            R(lambda e: e.tensor_reduce(out=m2, in_=msk, axis=AX.X, op=ALU.max))
            R(lambda e: e.tensor_tensor(out=oh2, in0=msk, in1=bc(m2, 8), op=ALU.is_equal))
            R(lambda e: e.tensor_tensor(out=dd, in0=m2, in1=m1, op=ALU.subtract))
            R(lambda e: e.activation(out=ed, in_=dd, func=AF.Exp), "act")
            R(lambda e: e.tensor_scalar(out=den, in0=ed, scalar1=1.0, scalar2=None, op0=ALU.add))
            R(lambda e: e.reciprocal(out=den, in_=den))
            R(lambda e: e.tensor_tensor(out=wk[:, :, 0], in0=den, in1=ptop, op=ALU.mult), reads=[dcT])
            R(lambda e: e.tensor_tensor(out=wk[:, :, 1], in0=ed, in1=wk[:, :, 0], op=ALU.mult))
            for g in range(4):
                R(lambda e: e.tensor_tensor(out=ohA[:, :, 8 * g:8 * g + 8], in0=oh1, in1=bc(w4[:, 0, :, g], 8), op=ALU.mult))
                R(lambda e: e.tensor_tensor(out=ohB[:, :, 8 * g:8 * g + 8], in0=oh2, in1=bc(w4[:, 0, :, g], 8), op=ALU.mult))
            R(lambda e: e.memset(ohA[64:128, 8, :], 0.0))
            R(lambda e: e.memset(ohB[64:128, 8, :], 0.0))
            R(lambda e: e.memset(wk[64:128, 8, :], 0.0))
            R(lambda e: e.tensor_tensor(out=sel[:], in0=ohA[:], in1=ohB[:], op=ALU.add))
            k.op("dve", lambda e: e.tensor_tensor(out=comb[:], in0=ohA[:], in1=bc(wk[:, :, 0], 32), op=ALU.mult),
                 reads=[rtT], writes=[combT])
            k.op("dve", lambda e: e.tensor_tensor(out=lgA[:, :, 0:32], in0=ohB[:], in1=bc(wk[:, :, 1], 32), op=ALU.mult),
                 reads=[rtT], writes=[rtT])
            k.op("dve", lambda e: e.tensor_tensor(out=comb[:], in0=comb[:], in1=lgA[:, :, 0:32], op=ALU.add),
                 reads=[rtT], writes=[combT])
            tmp32 = sb("tmp32", [128, 9, 32], F32, s3)
            prod = sb("prod", [128, 9, 32], F32, s3)
            flg = sb("flg", [128, 2], F32, s3)

            def mmp(e):
                ins = None
                for tt in range(9):
                    for t2 in range(tt):
                        ins = e.matmul(ps[6][:, tt * 32:(tt + 1) * 32], lhsT=ones_bf[:], rhs=sel[:, t2, :], start=(t2 == 0), stop=False)
                    ins = e.matmul(ps[6][:, tt * 32:(tt + 1) * 32], lhsT=Uu[:], rhs=sel[:, tt, :], start=(tt == 0), stop=True)
                return ins
            k.op("pe", mmp, reads=[rtT, dcT], writes=[pst[6]])
            pos = ps[6][:, 0:288].rearrange("p (a b) -> p a b", b=32)
            R(lambda e: e.tensor_scalar(out=prod[:], in0=pos, scalar1=float(CAP), scalar2=BIGSLOT, op0=ALU.is_ge, op1=ALU.mult),
              reads=[pst[6]])
            R(lambda e: e.tensor_tensor(out=tmp32[:], in0=pos, in1=base32[:].unsqueeze(1).to_broadcast([128, 9, 32]), op=ALU.add),
              reads=[pst[6], dcT])
            R(lambda e: e.tensor_tensor(out=tmp32[:], in0=tmp32[:], in1=prod[:], op=ALU.add))
            for kk, oh in enumerate((ohA, ohB)):
                R(lambda e: e.tensor_tensor(out=prod[:], in0=oh[:], in1=tmp32[:], op=ALU.mult))
                R(lambda e: e.tensor_reduce(out=slotf[:, :, kk], in_=prod[:], axis=AX.X, op=ALU.add))
            R(lambda e: e.memset(slotf[64:128, 8, :], BIGSLOT))
            R(lambda e: e.tensor_copy(out=sloti[:], in_=slotf[:]))
            R(lambda e: e.tensor_scalar(out=slotf[:], in0=slotf[:], scalar1=BIGSLOT, scalar2=None, op0=ALU.is_ge))
            R(lambda e: e.memset(slotf[64:128, 8, :], 0.0))
            R(lambda e: e.tensor_reduce(out=flg[:, 0:1], in_=slotf[:].rearrange("p a b -> p (a b)"), axis=AX.X, op=ALU.add))
            k.op("pe", lambda e: e.matmul(ps[5][0:1, 0:1], lhsT=flg[:, 0:1], rhs=ones_f[:, 0:1], start=True, stop=True),
                 reads=[rtT], writes=[pst[5]])
            k.op("dve", lambda e: e.tensor_copy(out=flagi[0:1, 0:1], in_=ps[5][0:1, 0:1]), reads=[pst[5]], writes=[dispT])
            for tt in range(9):
                for kk in range(2):
                    k.dma("pool", None, None, reads=[hbsT[tt], rtT], writes=[XsT] + XsZ,
                          fn=lambda e: e.indirect_dma_start(out=Xs[:, :], out_offset=bass.IndirectOffsetOnAxis(ap=sloti[:, tt, kk:kk + 1], axis=0),
                                                            in_=hbs[tt][:], in_offset=None, bounds_check=NE * CAP - 1, oob_is_err=False))
            precast(1000)
            k.phase_end()

        stW = ExitStack()
        slot3 = sb("wslot2", [128, NK, 512], BF16, stW)
        gu = [wslots[0], wslots[1], slot3]
        guT = [wslotT[0], wslotT[1], T()]
        wd = [sb("wd%d" % i, [128, 4, D], BF16, stW) for i in range(2)]
        wdT = [T() for _ in range(2)]
        gctr = [0]

        def load_expert(ex, parts="gud"):
            sgi = (2 * ex) % 3
            sui = (2 * ex + 1) % 3
            di = ex % 2
            if ex < NPRE:
                q_ = "sp"
                sg_, su_, sd_ = wg_bf[ex], wu_bf[ex], wd_bf[ex]
            else:
                q_ = "pool"
                sg_ = w_gate[ex].rearrange("(p kc) f -> p kc f", kc=NK)
                su_ = w_up[ex].rearrange("(p kc) f -> p kc f", kc=NK)
                sd_ = w_down[ex].rearrange("(fc p) d -> p fc d", p=128)
            kw_ = {} if ex < NPRE else {"max_dma_last_dim": 4096}
            if "g" in parts:
                k.dma(q_, gu[sgi][:], sg_, reads=[pcT[ex][0]], writes=[guT[sgi]], **kw_)
            if "u" in parts:
                k.dma(q_, gu[sui][:], su_, reads=[pcT[ex][1]], writes=[guT[sui]], **kw_)
            if "d" in parts:
                k.dma(q_, wd[di][:], sd_, reads=[pcT[ex][2]], writes=[wdT[di]])
            return sgi, sui, di

        with ExitStack() as s4:
            Xe2 = [sb("Xe%d" % i, [128, D], BF16, s4) for i in range(2)]
            XeTk2 = [T(), T()]
            XeT_ = sb("XeT", [128, NK, CAP], BF16, s4)
            hidS = sb("hidS", [128, 4, CAP], BF16, s4)
            hidTM = sb("hidTM", [128, 512], BF16, s4)
            hidTMT = T()
            Yo = sb("Yo", [128, D], BF16, s4)
            XeTT, hidST, YoT = T(), T(), T()
            sgW, sgWT = Yo[:, 0:1024].bitcast(F32), YoT
            precast(1000)
            load_expert(0)
            k.dma("sp", Xe2[0][:], Xs[0:CAP, :], reads=[XsT], writes=[XeTk2[0]])

            def emit_T(ex):
                Xe, XeTk = Xe2[ex % 2], XeTk2[ex % 2]
                pbf = ps[0][:].bitcast(BF16)
                for half in range(2):
                    def tr(e):
                        ins = None
                        for j in range(8):
                            kc = half * 8 + j
                            ins = e.transpose(out=pbf[:, j * 128:(j + 1) * 128], in_=Xe[:, kc::NK], identity=ident_bf[:])
                        return ins
                    k.op("pe", tr, reads=[XeTk], writes=[pst[0]])
                    k.op("act", lambda e: e.copy(out=XeT_[:, half * 8:half * 8 + 8, :], in_=pbf.rearrange("p (j t) -> p j t", t=128)),
                         reads=[pst[0]], writes=[XeTT])

            emit_T(0)
            for ex in range(NE):
                sgi, sui, di = load_expert(ex, "")
                if ex + 1 < NE:
                    k.dma("sp", Xe2[(ex + 1) % 2][:], Xs[(ex + 1) * CAP:(ex + 2) * CAP, :], reads=[XsT], writes=[XeTk2[(ex + 1) % 2]])
                    load_expert(ex + 1, "gd")
                bq = 2 + 2 * (ex % 2)

                def mmgu(e):
                    ins = None
                    for kc in range(NK):
                        e.matmul(ps[bq][:, :], lhsT=XeT_[:, kc, :], rhs=gu[sgi][:, kc, :], start=(kc == 0), stop=(kc == NK - 1))
                        ins = e.matmul(ps[bq + 1][:, :], lhsT=XeT_[:, kc, :], rhs=gu[sui][:, kc, :], start=(kc == 0),
                                       stop=(kc == NK - 1))
                    return ins
                k.op("pe", mmgu, reads=[XeTT, guT[sgi], guT[sui]], writes=[pst[bq], pst[bq + 1]])
                k.op("act", lambda e: e.activation(out=sgW, in_=ps[bq][:, :], func=AF.Silu), reads=[pst[bq]], writes=[sgWT])
                k.op("dve", lambda e: e.tensor_tensor(out=hidTM[:], in0=sgW, in1=ps[bq + 1][:, :], op=ALU.mult),
                     reads=[sgWT, pst[bq + 1]], writes=[hidTMT])
                if ex + 1 < NE:
                    load_expert(ex + 1, "u")
                    emit_T(ex + 1)
                pbfh = ps[1][:].bitcast(BF16)

                def trh(e):
                    ins = None
                    for fc in range(4):
                        ins = e.transpose(out=pbfh[:, fc * 128:(fc + 1) * 128], in_=hidTM[:, fc * 128:(fc + 1) * 128], identity=ident_bf[:])
                    return ins
                k.op("pe", trh, reads=[hidTMT], writes=[pst[1]])
                k.op("act", lambda e: e.copy(out=hidS[:], in_=pbfh[:, 0:512].rearrange("p (j t) -> p j t", t=128)),
                     reads=[pst[1]], writes=[hidST])
                for db in range(4):
                    py = 6 + db % 2

                    def mmd(e):
                        ins = None
                        for fc in range(4):
                            ins = e.matmul(ps[py][:, :], lhsT=hidS[:, fc, :], rhs=wd[di][:, fc, db * 512:(db + 1) * 512],
                                           start=(fc == 0), stop=(fc == 3))
                        return ins
                    k.op("pe", mmd, reads=[hidST, wdT[di]], writes=[pst[py]])
                    k.op("act", lambda e: e.copy(out=Yo[:, db * 512:(db + 1) * 512], in_=ps[py][:, :]), reads=[pst[py]], writes=[YoT])
                k.dma("act", Ys[ex * CAP:(ex + 1) * CAP, :], Yo[:], reads=[YoT], writes=[YsT])
            k.soft_barrier()

        def final_store(views):
            nfb, ot0, ot1, junk = views
            nfbT = T()
            ot = [ot0, ot1]
            otT = [T(), T()]
            junkT = T()
            ssT = T()
            k.dma("sp", nfb, nfin[:, :], writes=[nfbT])
            for tt, (r0, nr) in enumerate(TT):
                b2 = tt % 2
                k.op("act", lambda e: e.activation(out=junk[0:nr, :], in_=acc[0:nr, tt, :], func=AF.Square, accum_out=ssE[0:nr, 0:1]),
                     reads=[accT[tt]], writes=[junkT, ssT])
                k.op("act", lambda e: e.activation(out=ssE[0:nr, 1:2], in_=ssE[0:nr, 0:1], func=AF.Sqrt, scale=1.0 / D,
                                                   bias=pv_eps[0:nr, :]), writes=[ssT])
                k.op("dve", lambda e: e.reciprocal(out=ssE[0:nr, 2:3], in_=ssE[0:nr, 1:2]), writes=[ssT])
                k.op("dve", lambda e: e.scalar_tensor_tensor(out=ot[b2][0:nr, :], in0=acc[0:nr, tt, :], scalar=ssE[0:nr, 2:3],
                                                             in1=nfb[0:nr, :], op0=ALU.mult, op1=ALU.mult),
                     reads=[accT[tt], ssT, nfbT], writes=[otT[b2]])
                k.dma("sp", y_main[r0:r0 + nr, :], ot[b2][0:nr, :], reads=[otT[b2]])
            k.drain_dmas("sp")

        ssE = sb("ssE", [128, 4], F32, stW)
        wdf = [w_[:].rearrange("p a b -> p (a b)").bitcast(F32) for w_ in wd]
        s3f = slot3[:].rearrange("p a b -> p (a b)")
        regs = {e: stW.enter_context(k.eng[e].register("rf_" + e)) for e in ("pe", "act", "dve", "pool", "sp")}
        for e in regs:
            k._wait(e, [dispT.w])
            k.eng[e].reg_load(regs[e], flagi[0:1, 0:1])
        snap = k.snapshot()
        if FORCE_FALLBACK:
            cmpv = 7
        else:
            cmpv = 0
        k.start_defer()
        if True:
            wd0 = wd[0][:].rearrange("p a b -> p (a b)")
            G = [wd0[:, 0:D], wd0[:, D:2 * D]]
            GT = [T(), T()]
            for tt, (r0, nr) in enumerate(TT):
                for kk in range(2):
                    k.dma("pool", None, None, reads=[YsT], writes=[GT[kk]],
                          fn=lambda e: e.indirect_dma_start(out=G[kk], out_offset=None, in_=Ys[:, :],
                                                            in_offset=bass.IndirectOffsetOnAxis(ap=sloti[:, tt, kk:kk + 1], axis=0),
                                                            bounds_check=NE * CAP - 1, oob_is_err=False))
                    k.op("dve", lambda e: e.scalar_tensor_tensor(out=acc[0:nr, tt, :], in0=G[kk][0:nr, :], scalar=wk[0:nr, tt, kk:kk + 1],
                                                                 in1=acc[0:nr, tt, :], op0=ALU.mult, op1=ALU.add),
                         reads=[GT[kk]], writes=[accT[tt]])
            final_store((wdf[1][:, 0:D], wdf[1][:, D:2 * D], s3f[:, 0:2 * D].bitcast(F32), s3f[:, 2 * D:3 * D]))
        listA = k.end_defer()
        k.restore(snap)
        k.start_defer()
        if True:
            with ExitStack() as s4:
                hid = sb("hid", [128, 4, TV], BF16, s4)
                hidT = T()
                sgt = [sb("sgt%d" % i, [128, 512], F32, s4) for i in range(2)]
                sgtT = [T() for _ in range(2)]
                mctr = 0
                yctr = 0
                for ex in range(NE):
                    sgi, sui, di = load_expert(ex)
                    for (t0, n) in NT:
                        for fc in range(4):
                            pa = mctr % 2
                            pu = 2 + mctr % 2
                            mctr += 1

                            def mmg_(e):
                                ins = None
                                for kc in range(NK):
                                    ins = e.matmul(ps[pa][:, 0:n], lhsT=gu[sgi][:, kc, fc * 128:(fc + 1) * 128],
                                                   rhs=h2T[:, kc, t0:t0 + n], start=(kc == 0), stop=(kc == NK - 1))
                                return ins

                            def mmu_(e):
                                ins = None
                                for kc in range(NK):
                                    ins = e.matmul(ps[pu][:, 0:n], lhsT=gu[sui][:, kc, fc * 128:(fc + 1) * 128],
                                                   rhs=h2T[:, kc, t0:t0 + n], start=(kc == 0), stop=(kc == NK - 1))
                                return ins
                            k.op("pe", mmg_, reads=[h2Tt, guT[sgi]], writes=[pst[pa]])
                            k.op("pe", mmu_, reads=[h2Tt, guT[sui]], writes=[pst[pu]])
                            s_ = pa
                            k.op("act", lambda e: e.activation(out=sgt[s_][:, 0:n], in_=ps[pa][:, 0:n], func=AF.Silu),
                                 reads=[pst[pa]], writes=[sgtT[s_]])
                            k.op("dve", lambda e: e.tensor_tensor(out=hid[:, fc, t0:t0 + n], in0=sgt[s_][:, 0:n], in1=ps[pu][:, 0:n],
                                                                  op=ALU.mult), reads=[sgtT[s_], pst[pu]], writes=[hidT])
                    for tt, (r0, nr) in enumerate(TT):
                        for db in range(4):
                            py = 4 + yctr % 4
                            yctr += 1

                            def mmd(e):
                                ins = None
                                for fc in range(4):
                                    ins = e.matmul(ps[py][0:nr, :], lhsT=hid[:, fc, r0:r0 + nr], rhs=wd[di][:, fc, db * 512:(db + 1) * 512],
                                                   start=(fc == 0), stop=(fc == 3))
                                return ins
                            k.op("pe", mmd, reads=[hidT, wdT[di]], writes=[pst[py]])
                            k.op("dve", lambda e: e.scalar_tensor_tensor(out=acc[0:nr, tt, db * 512:(db + 1) * 512], in0=ps[py][0:nr, :],
                                                                         scalar=comb[0:nr, tt, ex:ex + 1],
                                                                         in1=acc[0:nr, tt, db * 512:(db + 1) * 512],
                                                                         op0=ALU.mult, op1=ALU.add),
                                 reads=[pst[py], combT], writes=[accT[tt]])
                k.soft_barrier()
            final_store((wdf[1][:, 0:D], wdf[1][:, D:2 * D], wdf[0][:, 0:D], s3f[:, 2 * D:3 * D]))
        listB = k.end_defer()
        for e in regs:
            if not listA[e] and not listB[e]:
                continue
            with k.eng[e].If_eq(regs[e], cmpv):
                k.replay(e, listA[e])
            with k.eng[e].Else():
                k.replay(e, listB[e])
        es.pop_all()
    return nc


def _pvec(inp):
    def fm(v, n):
        return np.ascontiguousarray(np.asarray(v, np.float32).reshape(n, 128).T)
    wdw = np.asarray(inp["w_dw"][0], np.float32)
    wdw_fm = np.ascontiguousarray(wdw.reshape(CONVW, 8, 128).transpose(2, 1, 0)).reshape(128, 8 * CONVW)
    cols = [fm(inp["norm_mix"][0], 16), fm(inp["norm_ffn"][0], 16), fm(inp["b_dw"][0], 8), fm(inp["ln_conv_g"][0], 8),
            fm(inp["ln_conv_b"][0], 8), fm(inp["lb_logits"][0], 8), fm(inp["lb_logits"][1], 8),
            np.asarray(inp["hgrn_norm_g"][0], np.float32).reshape(128, 1), wdw_fm]
    return np.ascontiguousarray(np.concatenate(cols, axis=1))


_NC_CACHE = {}
STAGE = 9


def kernel(**inp):
    inp = {k_: np.asarray(v) for k_, v in inp.items()}
    if "nc" not in _NC_CACHE:
        _NC_CACHE["nc"] = build_program(STAGE)
    nc = _NC_CACHE["nc"]
    xp = inp["x_prompt"].astype(np.float32, copy=False)
    xs = inp["x_sample"].astype(np.float32, copy=False).reshape(128 * 4, D)
    pv = _pvec(inp)
    w_r = np.ascontiguousarray(np.concatenate([inp["w_router_group"][0], inp["w_router_expert"][0]], axis=1))
    b_r = np.ascontiguousarray(np.broadcast_to(np.concatenate([inp["b_router_group"][0], inp["b_router_expert"][0]])[None, :], (128, 36)))
    shared = {
        "pvec": pv, "nfin": np.ascontiguousarray(np.broadcast_to(inp["norm_final"][None, :], (128, D))),
        "gffn_rep": np.ascontiguousarray(np.broadcast_to(inp["norm_ffn"][0][None, :], (128, D))),
        "zeros_d": np.zeros((128, D), ml_dtypes.bfloat16),
        "w_in": np.ascontiguousarray(inp["w_in"][0]), "w_out": np.ascontiguousarray(inp["w_out"][0]),
        "w_r": w_r, "b_r": b_r,
    }
    if True:
        shared["w_gate"] = np.ascontiguousarray(inp["w_exp_gate"][0])
        shared["w_up"] = np.ascontiguousarray(inp["w_exp_up"][0])
        shared["w_down"] = np.ascontiguousarray(inp["w_exp_down"][0])
    in_maps = []
    for c in range(NCORES):
        b, hf = c // 2, c % 2
        cur = xp[b, hf * 1024:(hf + 1) * 1024]
        if hf == 1:
            prev = xp[b, 0:1024]
        else:
            prev = np.zeros((1024, D), np.float32)
        xm = np.concatenate([cur, xs[c * 64:(c + 1) * 64], prev[1024 - THALO:]], axis=0)
        m = dict(shared)
        m["x_main"] = np.ascontiguousarray(xm)
        m["x_prev"] = np.ascontiguousarray(prev)
        m["sconv"] = np.ascontiguousarray(inp["state_conv"][0, c * 16:(c + 1) * 16])
        m["shgrn"] = np.ascontiguousarray(inp["state_hgrn"][0, c * 16:(c + 1) * 16])
        in_maps.append(m)
    res = run_bass_kernel_spmd(nc, in_maps, core_ids=list(range(NCORES)))
    R = res.results
    y_prompt = np.zeros((4, 2048, D), np.float32)
    y_sample = np.zeros((128, 4, D), np.float32)
    ncp = np.zeros((1, 4, 30, 1024), np.float32)
    nhp = np.zeros((1, 4, 8, 128, 128), np.float32)
    ncs = np.zeros((1, 128, 30, 1024), np.float32)
    nhs = np.zeros((1, 128, 8, 128, 128), np.float32)
    for c in range(NCORES):
        b, hf = c // 2, c % 2
        ym = R[c]["y_main"]
        y_prompt[b, hf * 1024:(hf + 1) * 1024] = ym[0:1024]
        y_sample[c * 16:(c + 1) * 16] = ym[1024:1088].reshape(16, 4, D)
        if hf == 1:
            ncp[0, b] = R[c]["conv_p"]
            nhp[0, b] = R[c]["hgrn_p"]
        ncs[0, c * 16:(c + 1) * 16] = R[c]["conv_s"]
        nhs[0, c * 16:(c + 1) * 16] = R[c]["hgrn_s"]
    return (y_prompt, y_sample, ncp, nhp, ncs, nhs)
```
